# Optimizing a Trainium2 kernel written in Bass

```python
import math
import jax
import jax.numpy as jnp
from jax import lax
import numpy as np

D_MODEL = 2048
BATCH = 4
SEQ = 8192
DEPTH = 2

GRID_W = 64
CTX_LEN = 256

MLSTM_HEADS = 4
MLSTM_HD = 128
MLSTM_W = MLSTM_HEADS * MLSTM_HD
MLSTM_CHUNK = 64
MLSTM_CONV = 3
CONV_W = 512
CONV_K = 31
GQA_HEADS = 4
GQA_KV_HEADS = 2
GQA_HD = 128
DIFF_HEADS = 4
DIFF_HD = 64
DIFF_VD = 2 * DIFF_HD

BRANCH_W = 512
N_BRANCHES = 4
Q_BLOCK = 128
ROPE_THETA = 10000.0

D_FF = 5632
N_EXPERTS = 8
TOP_K = 2
D_FF_EXPERT = 7168
MOE_BLOCK = 128

EPS = 1e-6

IN_SPLITS = (2 * MLSTM_W, MLSTM_W, MLSTM_W, 4 * MLSTM_HEADS, 2 * CONV_W, GQA_HEADS * GQA_HD, GQA_KV_HEADS * GQA_HD, GQA_KV_HEADS * GQA_HD, DIFF_HEADS * 2 * DIFF_HD, DIFF_HEADS * 2 * DIFF_HD, DIFF_HEADS * DIFF_VD)
D_IN = sum(IN_SPLITS)

kernel_name = 'hybrid_flow_block'


def rms_norm(x, g):
    xf = x.astype(jnp.float32)
    y = xf * lax.rsqrt(jnp.mean(jnp.square(xf), axis=-1, keepdims=True) + EPS)
    return (y * g.astype(jnp.float32)).astype(x.dtype)


def layer_norm(x, g, b):
    xf = x.astype(jnp.float32)
    mu = jnp.mean(xf, axis=-1, keepdims=True)
    xc = xf - mu
    y = xc * lax.rsqrt(jnp.mean(jnp.square(xc), axis=-1, keepdims=True) + EPS)
    return (y * g.astype(jnp.float32) + b.astype(jnp.float32)).astype(x.dtype)


def modulate(h, shift, scale):
    return h * (1.0 + scale) + shift


def split_cols(z):
    return jnp.split(z, np.cumsum(IN_SPLITS)[:-1].tolist(), axis=-1)


def split_heads(a, n_heads):
    B, T, W = a.shape
    return a.reshape(B, T, n_heads, W // n_heads).transpose(0, 2, 1, 3)


def merge_heads(a):
    B, H, T, d = a.shape
    return a.transpose(0, 2, 1, 3).reshape(B, T, H * d)


def depthwise_conv(x, w, b):
    pad = w.shape[0] // 2
    y = lax.conv_general_dilated(x, w[:, None, :], window_strides=(1,), padding=[(pad, w.shape[0] - 1 - pad)], dimension_numbers=('NWC', 'WIO', 'NWC'), feature_group_count=x.shape[-1])
    return y + b


def axial_angles(rows, cols, head_dim):
    r = head_dim // 4
    freqs = ROPE_THETA ** (-jnp.arange(r, dtype=jnp.float32) / r)
    return jnp.concatenate([rows[:, None].astype(jnp.float32) * freqs, cols[:, None].astype(jnp.float32) * freqs], axis=-1)


def axial_rope(x, ang):
    *lead, T, d = x.shape
    r = d // 4
    xs = x.astype(jnp.float32).reshape(*lead, T, 2, 2, r)
    a, b = xs[..., 0, :], xs[..., 1, :]
    ang = ang.reshape(T, 2, r)
    cos, sin = jnp.cos(ang), jnp.sin(ang)
    out = jnp.stack([a * cos - b * sin, a * sin + b * cos], axis=-2)
    return out.reshape(x.shape).astype(x.dtype)


def attend_latent(q, k, v, k_ctx, v_ctx):
    B, Hq, S, d = q.shape
    Hk = k.shape[1]
    G = Hq // Hk
    nb = S // Q_BLOCK
    scale = d ** -0.5
    qb = q.reshape(B, Hk, G, nb, Q_BLOCK, d).transpose(3, 0, 1, 2, 4, 5)

    def block(qi):
        s = jnp.concatenate([jnp.einsum('bhgqd,bhkd->bhgqk', qi, k), jnp.einsum('bhgqd,bhkd->bhgqk', qi, k_ctx)], axis=-1)
        p = jax.nn.softmax(s.astype(jnp.float32) * scale, axis=-1).astype(v.dtype)
        return jnp.einsum('bhgqk,bhkv->bhgqv', p[..., :S], v) + jnp.einsum('bhgqk,bhkv->bhgqv', p[..., S:], v_ctx)

    o = lax.map(block, qb)
    return o.transpose(1, 2, 3, 0, 4, 5).reshape(B, Hq, S, v.shape[-1])


def attend_ctx(q, k, v):
    B, Hq, T, d = q.shape
    Hk = k.shape[1]
    qg = q.reshape(B, Hk, Hq // Hk, T, d)
    s = jnp.einsum('bhgqd,bhkd->bhgqk', qg, k).astype(jnp.float32) * d ** -0.5
    p = jax.nn.softmax(s, axis=-1).astype(v.dtype)
    return jnp.einsum('bhgqk,bhkv->bhgqv', p, v).reshape(B, Hq, T, v.shape[-1])


def mlstm_chunkwise(q, k, v, log_i, log_f):
    f32 = jnp.float32
    q, k, v, log_i, log_f = (a.astype(f32) for a in (q, k, v, log_i, log_f))
    B, H, T, dh = q.shape
    n_chunks = T // MLSTM_CHUNK

    def chunks(a):
        return jnp.moveaxis(a.reshape(B, H, n_chunks, MLSTM_CHUNK, *a.shape[3:]), 2, 0)

    lower = jnp.tril(jnp.ones((MLSTM_CHUNK, MLSTM_CHUNK), dtype=bool))

    def step(carry, blk):
        c_state, n_state, m_state = carry
        qc, kc, vc, ic, fc = blk
        b = jnp.cumsum(fc, axis=-1)
        inter = b + m_state[..., None]
        d_log = jnp.where(lower, b[..., :, None] - b[..., None, :] + ic[..., None, :], -jnp.inf)
        m_t = jnp.maximum(inter, jnp.max(d_log, axis=-1))
        w = jnp.exp(d_log - m_t[..., None]) * jnp.einsum('bhtd,bhsd->bhts', qc, kc)
        a_inter = jnp.exp(inter - m_t)
        num = a_inter[..., None] * jnp.einsum('bhvk,bhtk->bhtv', c_state, qc) + jnp.einsum('bhts,bhsv->bhtv', w, vc)
        den = a_inter * jnp.einsum('bhk,bhtk->bht', n_state, qc) + jnp.sum(w, axis=-1)
        h = num / jnp.maximum(jnp.abs(den), jnp.exp(-m_t))[..., None]
        b_end = b[..., -1]
        g = b_end[..., None] - b + ic
        m_new = jnp.maximum(b_end + m_state, jnp.max(g, axis=-1))
        decay = jnp.exp(b_end + m_state - m_new)
        wg = jnp.exp(g - m_new[..., None])
        c_state = decay[..., None, None] * c_state + jnp.einsum('bhs,bhsv,bhsk->bhvk', wg, vc, kc)
        n_state = decay[..., None] * n_state + jnp.einsum('bhs,bhsk->bhk', wg, kc)
        return (c_state, n_state, m_new), h

    init = (jnp.zeros((B, H, dh, dh), f32), jnp.zeros((B, H, dh), f32), jnp.zeros((B, H), f32))
    _, hs = lax.scan(step, init, tuple(chunks(a) for a in (q, k, v, log_i, log_f)))
    return jnp.moveaxis(hs, 0, 2).reshape(B, H, T, dh)


def mlstm_inputs(z, conv_w, conv_b, gate_b):
    qk = jax.nn.silu(depthwise_conv(z[0], conv_w, conv_b))
    q, k = jnp.split(qk, 2, axis=-1)
    B, T, _ = q.shape
    gates = (z[3] + gate_b).astype(jnp.float32).reshape(B, T, 4, MLSTM_HEADS).transpose(2, 0, 3, 1)
    log_gates = (gates[0], jax.nn.log_sigmoid(gates[1]), gates[2], jax.nn.log_sigmoid(gates[3]))
    return split_heads(q, MLSTM_HEADS), split_heads(k, MLSTM_HEADS) * MLSTM_HD ** -0.5, split_heads(z[1], MLSTM_HEADS), log_gates


def mlstm_output(h, o_pre, norm_g):
    hn = rms_norm(h, norm_g.reshape(MLSTM_HEADS, 1, MLSTM_HD))
    return merge_heads(hn).astype(o_pre.dtype) * jax.nn.sigmoid(o_pre)


def conformer_conv(glu_pre, w, b, ln_g, ln_b):
    a, gt = jnp.split(glu_pre, 2, axis=-1)
    u = depthwise_conv(a * jax.nn.sigmoid(gt), w, b)
    return jax.nn.silu(layer_norm(u, ln_g, ln_b))


def gqa_heads(z, q_g, k_g):
    q = rms_norm(split_heads(z[5], GQA_HEADS), q_g)
    k = rms_norm(split_heads(z[6], GQA_KV_HEADS), k_g)
    return q, k, split_heads(z[7], GQA_KV_HEADS)


def diff_heads(z, q_g, k_g):
    B, T, _ = z[8].shape
    q = rms_norm(z[8].reshape(B, T, DIFF_HEADS, 2, DIFF_HD), q_g).transpose(3, 0, 2, 1, 4)
    k = rms_norm(z[9].reshape(B, T, DIFF_HEADS, 2, DIFF_HD), k_g).transpose(3, 0, 2, 1, 4)
    return q[0], q[1], k[0], k[1], split_heads(z[10], DIFF_HEADS)


def diff_output(o, sub_g, lam_init):
    return merge_heads(rms_norm(o, sub_g) * (1.0 - lam_init))


def merge_branches(h, branches, mg_w, mg_b, br_w, o_w):
    y = None
    for j, o in enumerate(branches):
        term = jax.nn.sigmoid(h @ mg_w[j] + mg_b[j]) * (o.astype(h.dtype) @ br_w[j])
        y = term if y is None else y + term
    return y @ o_w


def hybrid_mixer(h_lat, h_ctx, need_ctx, ang_gqa, ang_diff, lam_init, w_in, m_conv_w, m_conv_b, m_gate_b, m_norm_g, c_dw_w, c_dw_b, c_ln_g, c_ln_b, g_q_g, g_k_g, d_q_g, d_k_g, d_lam, d_sub_g, mg_w, mg_b, br_w, o_w):
    L = h_ctx.shape[1]
    zl = split_cols(h_lat @ w_in)
    zc = split_cols(h_ctx @ w_in)
    cat = lambda a, b: jnp.concatenate([a, b], axis=2)
    rev = lambda a: jnp.flip(a, axis=2)

    ql, kl, vl, gl = mlstm_inputs(zl, m_conv_w, m_conv_b, m_gate_b)
    qc, kc, vc, gc = mlstm_inputs(zc, m_conv_w, m_conv_b, m_gate_b)
    h_fwd = mlstm_chunkwise(cat(qc, ql), cat(kc, kl), cat(vc, vl), cat(gc[0], gl[0]), cat(gc[1], gl[1]))
    h_bwd = mlstm_chunkwise(cat(rev(qc), rev(ql)), cat(rev(kc), rev(kl)), cat(rev(vc), rev(vl)), cat(rev(gc[2]), rev(gl[2])), cat(rev(gc[3]), rev(gl[3])))
    a_lat = mlstm_output(h_fwd[:, :, L:] + rev(h_bwd[:, :, L:]), zl[2], m_norm_g)

    b_lat = conformer_conv(zl[4], c_dw_w, c_dw_b, c_ln_g, c_ln_b)

    gq_l, gk_l, gv_l = gqa_heads(zl, g_q_g, g_k_g)
    gq_c, gk_c, gv_c = gqa_heads(zc, g_q_g, g_k_g)
    c_lat = merge_heads(attend_latent(axial_rope(gq_l, ang_gqa), axial_rope(gk_l, ang_gqa), gv_l, gk_c, gv_c))

    dl = d_lam.astype(jnp.float32)
    lam = jnp.exp(jnp.sum(dl[0] * dl[1])) - jnp.exp(jnp.sum(dl[2] * dl[3])) + lam_init
    dq1_l, dq2_l, dk1_l, dk2_l, dv_l = diff_heads(zl, d_q_g, d_k_g)
    dq1_c, dq2_c, dk1_c, dk2_c, dv_c = diff_heads(zc, d_q_g, d_k_g)
    o1 = attend_latent(axial_rope(dq1_l, ang_diff), axial_rope(dk1_l, ang_diff), dv_l, dk1_c, dv_c)
    o2 = attend_latent(axial_rope(dq2_l, ang_diff), axial_rope(dk2_l, ang_diff), dv_l, dk2_c, dv_c)
    d_lat = diff_output(o1 - lam * o2, d_sub_g, lam_init)

    y_lat = merge_branches(h_lat, (a_lat, b_lat, c_lat, d_lat), mg_w, mg_b, br_w, o_w)
    if not need_ctx:
        return y_lat, None

    a_ctx = mlstm_output(h_fwd[:, :, :L] + rev(h_bwd[:, :, :L]), zc[2], m_norm_g)
    b_ctx = conformer_conv(zc[4], c_dw_w, c_dw_b, c_ln_g, c_ln_b)
    c_ctx_o = merge_heads(attend_ctx(gq_c, gk_c, gv_c))
    d_ctx = diff_output(attend_ctx(dq1_c, dk1_c, dv_c) - lam * attend_ctx(dq2_c, dk2_c, dv_c), d_sub_g, lam_init)
    y_ctx = merge_branches(h_ctx, (a_ctx, b_ctx, c_ctx_o, d_ctx), mg_w, mg_b, br_w, o_w)
    return y_lat, y_ctx


def swiglu(h, w1, w3, w2):
    return (jax.nn.silu(h @ w1) * (h @ w3)) @ w2


def moe_swiglu(t, router, w1, w3, w2):
    N = t.shape[0]
    n_assign = N * TOP_K
    logits = (t @ router).astype(jnp.float32)
    top_v, top_e = lax.top_k(logits, TOP_K)
    top_w = jax.nn.softmax(top_v, axis=-1)
    e_flat = top_e.reshape(-1)
    w_flat = top_w.reshape(-1)
    tok_flat = jnp.repeat(jnp.arange(N, dtype=jnp.int32), TOP_K)
    order = jnp.argsort(e_flat)
    e_s, tok_s, w_s = e_flat[order], tok_flat[order], w_flat[order]
    counts = jnp.bincount(e_flat, length=N_EXPERTS)
    padded = (counts + MOE_BLOCK - 1) // MOE_BLOCK * MOE_BLOCK
    pad_end = jnp.cumsum(padded)
    pad_start = pad_end - padded
    start = jnp.cumsum(counts) - counts
    dest = pad_start[e_s] + jnp.arange(n_assign, dtype=jnp.int32) - start[e_s]
    n_blocks = -(-n_assign // MOE_BLOCK) + N_EXPERTS
    P = n_blocks * MOE_BLOCK
    buf_tok = jnp.zeros((P,), jnp.int32).at[dest].set(tok_s)
    buf_w = jnp.zeros((P,), jnp.float32).at[dest].set(w_s)
    blk_e = jnp.minimum(jnp.searchsorted(pad_end, jnp.arange(n_blocks, dtype=jnp.int32) * MOE_BLOCK, side='right'), N_EXPERTS - 1)

    def step(y, blk):
        idx, wt, e = blk
        xb = t[idx]
        hb = jax.nn.silu(xb @ w1[e]) * (xb @ w3[e])
        return y.at[idx].add((hb @ w2[e]) * wt[:, None].astype(t.dtype)), None

    y, _ = lax.scan(step, jnp.zeros_like(t), (buf_tok.reshape(n_blocks, MOE_BLOCK), buf_w.reshape(n_blocks, MOE_BLOCK), blk_e))
    return y


def setup_inputs(seed: int = 0) -> dict:
    keys = iter(jax.random.split(jax.random.key(seed), 64))
    f32 = jnp.float32
    D = D_MODEL
    n_dense = (DEPTH + 1) // 2
    n_moe = DEPTH // 2

    def normal(shape, scale):
        return jax.random.normal(next(keys), shape, f32) * scale

    def gain(shape):
        return 1.0 + normal(shape, 0.02)

    f_bias = jnp.linspace(3.0, 6.0, MLSTM_HEADS, dtype=f32)
    mlstm_gate_b = jnp.concatenate([normal((DEPTH, MLSTM_HEADS), 0.1), f_bias + normal((DEPTH, MLSTM_HEADS), 0.1), normal((DEPTH, MLSTM_HEADS), 0.1), f_bias + normal((DEPTH, MLSTM_HEADS), 0.1)], axis=-1)
    return {
        'x': normal((BATCH, SEQ, D), 1.0),
        'c': normal((BATCH, D), 1.0),
        'ctx': normal((BATCH, CTX_LEN, D), 1.0),
        'c_ctx': normal((D,), 1.0),
        'ada_w': normal((DEPTH, D, 6 * D), 0.5 * D ** -0.5),
        'ada_b': normal((DEPTH, 6 * D), 0.01),
        'norm1_g': gain((DEPTH, D)),
        'norm2_g': gain((DEPTH, D)),
        'w_in': normal((DEPTH, D, D_IN), D ** -0.5),
        'mlstm_conv_w': normal((DEPTH, MLSTM_CONV, 2 * MLSTM_W), MLSTM_CONV ** -0.5),
        'mlstm_conv_b': normal((DEPTH, 2 * MLSTM_W), 0.01),
        'mlstm_gate_b': mlstm_gate_b,
        'mlstm_norm_g': gain((DEPTH, MLSTM_W)),
        'conv_dw_w': normal((DEPTH, CONV_K, CONV_W), CONV_K ** -0.5),
        'conv_dw_b': normal((DEPTH, CONV_W), 0.01),
        'conv_ln_g': gain((DEPTH, CONV_W)),
        'conv_ln_b': normal((DEPTH, CONV_W), 0.01),
        'gqa_q_norm_g': gain((DEPTH, GQA_HD)),
        'gqa_k_norm_g': gain((DEPTH, GQA_HD)),
        'diff_q_norm_g': gain((DEPTH, DIFF_HD)),
        'diff_k_norm_g': gain((DEPTH, DIFF_HD)),
        'diff_lambda': normal((DEPTH, 4, DIFF_HD), 0.1),
        'diff_subln_g': gain((DEPTH, DIFF_VD)),
        'merge_gate_w': normal((DEPTH, N_BRANCHES, D, D), D ** -0.5),
        'merge_gate_b': normal((DEPTH, N_BRANCHES, D), 0.01),
        'branch_w': normal((DEPTH, N_BRANCHES, BRANCH_W, D), BRANCH_W ** -0.5),
        'out_w': normal((DEPTH, D, D), D ** -0.5),
        'ffn_w1': normal((n_dense, D, D_FF), D ** -0.5),
        'ffn_w3': normal((n_dense, D, D_FF), D ** -0.5),
        'ffn_w2': normal((n_dense, D_FF, D), D_FF ** -0.5),
        'moe_router': normal((n_moe, D, N_EXPERTS), D ** -0.5),
        'moe_w1': normal((n_moe, N_EXPERTS, D, D_FF_EXPERT), D ** -0.5),
        'moe_w3': normal((n_moe, N_EXPERTS, D, D_FF_EXPERT), D ** -0.5),
        'moe_w2': normal((n_moe, N_EXPERTS, D_FF_EXPERT, D), D_FF_EXPERT ** -0.5),
    }


def reference(x, c, ctx, c_ctx, ada_w, ada_b, norm1_g, norm2_g, w_in, mlstm_conv_w, mlstm_conv_b, mlstm_gate_b, mlstm_norm_g, conv_dw_w, conv_dw_b, conv_ln_g, conv_ln_b, gqa_q_norm_g, gqa_k_norm_g, diff_q_norm_g, diff_k_norm_g, diff_lambda, diff_subln_g, merge_gate_w, merge_gate_b, branch_w, out_w, ffn_w1, ffn_w3, ffn_w2, moe_router, moe_w1, moe_w3, moe_w2):
    B, S, D = x.shape
    L = ctx.shape[1]
    ROWS = S // GRID_W
    rows = jnp.repeat(jnp.arange(ROWS, dtype=jnp.int32), GRID_W)
    cols = jnp.tile(jnp.arange(GRID_W, dtype=jnp.int32), ROWS)
    ang_gqa = axial_angles(rows, cols, GQA_HD)
    ang_diff = axial_angles(rows, cols, DIFF_HD)
    silu_c = jax.nn.silu(c)
    silu_cc = jax.nn.silu(c_ctx)
    x_lat, x_ctx = x, ctx
    for l in range(DEPTH):
        need_ctx = l < DEPTH - 1
        mod_l = jnp.split((silu_c @ ada_w[l] + ada_b[l])[:, None, :], 6, axis=-1)
        mod_c = jnp.split(silu_cc @ ada_w[l] + ada_b[l], 6, axis=-1)
        lam_init = 0.8 - 0.6 * math.exp(-0.3 * l)

        h_lat = modulate(rms_norm(x_lat, norm1_g[l]), mod_l[0], mod_l[1])
        h_ctx = modulate(rms_norm(x_ctx, norm1_g[l]), mod_c[0], mod_c[1])
        y_lat, y_ctx = hybrid_mixer(h_lat, h_ctx, need_ctx, ang_gqa, ang_diff, lam_init, w_in[l], mlstm_conv_w[l], mlstm_conv_b[l], mlstm_gate_b[l], mlstm_norm_g[l], conv_dw_w[l], conv_dw_b[l], conv_ln_g[l], conv_ln_b[l], gqa_q_norm_g[l], gqa_k_norm_g[l], diff_q_norm_g[l], diff_k_norm_g[l], diff_lambda[l], diff_subln_g[l], merge_gate_w[l], merge_gate_b[l], branch_w[l], out_w[l])
        x_lat = x_lat + mod_l[2] * y_lat
        if need_ctx:
            x_ctx = x_ctx + mod_c[2] * y_ctx

        h_lat = modulate(rms_norm(x_lat, norm2_g[l]), mod_l[3], mod_l[4])
        i = l // 2
        if l % 2 == 0:
            x_lat = x_lat + mod_l[5] * swiglu(h_lat, ffn_w1[i], ffn_w3[i], ffn_w2[i])
            if need_ctx:
                h_ctx = modulate(rms_norm(x_ctx, norm2_g[l]), mod_c[3], mod_c[4])
                x_ctx = x_ctx + mod_c[5] * swiglu(h_ctx, ffn_w1[i], ffn_w3[i], ffn_w2[i])
        else:
            tokens = h_lat.reshape(B * S, D)
            if need_ctx:
                h_ctx = modulate(rms_norm(x_ctx, norm2_g[l]), mod_c[3], mod_c[4])
                tokens = jnp.concatenate([tokens, h_ctx.reshape(B * L, D)], axis=0)
            f_out = moe_swiglu(tokens, moe_router[i], moe_w1[i], moe_w3[i], moe_w2[i])
            x_lat = x_lat + mod_l[5] * f_out[:B * S].reshape(B, S, D)
            if need_ctx:
                x_ctx = x_ctx + mod_c[5] * f_out[B * S:].reshape(B, L, D)
    return x_lat
```

```python
import math
from contextlib import ExitStack
import numpy as np
import concourse.bass as bass
import concourse.mybir as mybir
from concourse.bass_utils import run_bass_kernel_spmd

F32 = mybir.dt.float32
BF16 = mybir.dt.bfloat16
AF = mybir.ActivationFunctionType
ALU = mybir.AluOpType
AX = mybir.AxisListType

D = 2048
KC = 16
DIN = 5648
DFF = 5632
DFE = 7168
NE = 8
EPS = 1e-6
NFM = 3856
NTM = 1792
ENGS = ("pe", "dve", "act", "pool", "sp")


class Prog:
    def __init__(self, nc, es):
        self.nc = nc
        self.es = es
        self.q = {e: [] for e in ENGS}
        self.cnt = {e: 0 for e in ENGS}
        self.known = {e: {} for e in ENGS}
        self.track = {}
        self.sems = {}
        self.dma_cnt = {}
        self.free_sems = []
        for e in ("pe", "dve", "act", "pool"):
            self.sems[e] = es.enter_context(nc.semaphore("s_" + e))
        self.nsem = 0

    def _sem_for(self, key):
        if key not in self.sems:
            if self.free_sems:
                h, base = self.free_sems.pop()
            else:
                self.nsem += 1
                h, base = self.es.enter_context(self.nc.semaphore("d%d" % self.nsem)), 0
            self.sems[key] = h
            self.dma_cnt[key] = base
            for e in ENGS:
                self.known[e][key] = base
        return self.sems[key]

    def _deps(self, reads, writes):
        deps = []
        for k in reads:
            t = self.track.get(k)
            if t and t["w"]:
                deps.append(t["w"])
        for k in writes:
            t = self.track.get(k)
            if t:
                if t["w"]:
                    deps.append(t["w"])
                deps.extend(t["r"])
        return deps

    def _commit(self, reads, writes, me):
        for k in reads:
            t = self.track.setdefault(k, {"w": None, "r": []})
            t["r"] = [d for d in t["r"] if d[0] != me[0]] + [me]
        for k in writes:
            self.track[k] = {"w": me, "r": []}

    def _waits(self, eng, deps, skip_self=False):
        need = {}
        for (sk, v) in deps:
            if skip_self and sk == eng:
                continue
            if self.known[eng].get(sk, 0) >= v:
                continue
            if need.get(sk, 0) < v:
                need[sk] = v
        for sk, v in need.items():
            self.known[eng][sk] = v
        return list(need.items())

    def op(self, eng, fn, reads=(), writes=(), inc=True):
        deps = self._deps(reads, writes)
        waits = self._waits(eng, deps, skip_self=(eng == "pe"))
        if inc:
            self.cnt[eng] += 1
            me = (eng, self.cnt[eng])
            self.q[eng].append(([(self.sems[k], v) for k, v in waits], fn, (self.sems[eng], 1)))
        else:
            me = (eng, self.cnt[eng] + 1)
            self.q[eng].append(([(self.sems[k], v) for k, v in waits], fn, None))
        self._commit(reads, writes, me)

    def dma(self, eng, fn, chan, reads=(), writes=()):
        deps = self._deps(reads, writes)
        waits = self._waits(eng, deps)
        self._sem_for(chan)
        self.dma_cnt[chan] += 16
        me = (chan, self.dma_cnt[chan])
        self.q[eng].append(([(self.sems[k], v) for k, v in waits], fn, (self.sems[chan], 16)))
        self._commit(reads, writes, me)

    def _all_counts(self):
        final = {}
        for e in ("pe", "dve", "act", "pool"):
            if self.cnt[e]:
                final[e] = self.cnt[e]
        for sk, v in self.dma_cnt.items():
            if v:
                final[sk] = v
        return final

    def barrier(self):
        final = self._all_counts()
        for e in ENGS:
            waits = []
            for sk, v in final.items():
                if self.known[e].get(sk, 0) < v:
                    self.known[e][sk] = v
                    waits.append((sk, v))
            if waits:
                self.q[e].append(([(self.sems[k], v) for k, v in waits], None, None))
        self.track = {}
        for k in list(self.dma_cnt.keys()):
            self.free_sems.append((self.sems.pop(k), self.dma_cnt.pop(k)))
            for e in ENGS:
                self.known[e].pop(k, None)

    def emit(self):
        nc = self.nc
        self.barrier()
        q = self.q

        def run(engobj, name):
            for (waits, fn, inc) in q[name]:
                for (wh, wv) in waits:
                    engobj.wait_ge(wh, wv)
                if fn is not None:
                    ins = fn(engobj)
                    if inc is not None:
                        ins.then_inc(inc[0], inc[1])

        with nc.Block() as block:
            @block.tensor
            def _(e):
                run(e, "pe")

            @block.vector
            def _(e):
                run(e, "dve")

            @block.scalar
            def _(e):
                run(e, "act")

            @block.gpsimd
            def _(e):
                run(e, "pool")

            @block.sync
            def _(e):
                run(e, "sp")


class TPool:
    def __init__(self, B, es, name, shape, dt, n, psum=False):
        self.tiles = []
        for i in range(n):
            nm = "%s%d" % (name, i)
            if psum:
                t = es.enter_context(B.nc.psum_tensor(nm, list(shape), dt))
            else:
                t = es.enter_context(B.nc.sbuf_tensor(nm, list(shape), dt))
            self.tiles.append((t, nm))
        self.i = 0

    def get(self):
        t = self.tiles[self.i % len(self.tiles)]
        self.i += 1
        return t


class Builder:
    def __init__(self, S, L, depth=2, debug_out=None, stop=None, tiny=()):
        self.S, self.L, self.depth = S, L, depth
        self.stop = stop
        self.tiny = set(tiny)
        self.NT = S + L
        self.nc = bass.Bass("TRN2", target_bir_lowering=False)
        self.uid = 0
        self.debug_out = debug_out

    def mm(self, out, lhsT, rhs, start, stop, reads, writes, inc=None):
        self.P.op("pe", lambda e: e.matmul(out, lhsT=lhsT, rhs=rhs, start=start, stop=stop),
                  reads, writes, inc=bool(stop) if inc is None else bool(inc or stop))

    def tr(self, out, in_, ident, reads, writes):
        self.P.op("pe", lambda e: e.transpose(out=out, in_=in_, identity=ident), reads, writes)

    def act(self, out, in_, func, reads, writes, **kw):
        self.P.op("act", lambda e: e.activation(out=out, in_=in_, func=func, **kw), reads, writes)

    def tt(self, eng, out, in0, in1, op, reads, writes):
        self.P.op(eng, lambda e: e.tensor_tensor(out=out, in0=in0, in1=in1, op=op), reads, writes)

    def ts(self, eng, out, in0, s1, s2, op0, op1, reads, writes):
        if s2 is None:
            self.P.op(eng, lambda e: e.tensor_scalar(out=out, in0=in0, scalar1=s1, scalar2=None, op0=op0),
                      reads, writes)
        else:
            self.P.op(eng, lambda e: e.tensor_scalar(out=out, in0=in0, scalar1=s1, scalar2=s2, op0=op0, op1=op1),
                      reads, writes)

    def stt(self, eng, out, in0, scalar, in1, op0, op1, reads, writes):
        eng = "dve"
        self.P.op(eng, lambda e: e.scalar_tensor_tensor(out=out, in0=in0, scalar=scalar, in1=in1, op0=op0, op1=op1),
                  reads, writes)

    def cp(self, eng, out, in_, reads, writes):
        if eng == "act":
            self.P.op("act", lambda e: e.copy(out=out, in_=in_), reads, writes)
        else:
            self.P.op(eng, lambda e: e.tensor_copy(out=out, in_=in_), reads, writes)

    def memset(self, eng, ap, val, writes):
        self.P.op(eng, lambda e: e.memset(ap, val), (), writes)

    def recip(self, out, in_, reads, writes):
        self.P.op("dve", lambda e: e.reciprocal(out=out, in_=in_), reads, writes)

    def red(self, out, in_, op, reads, writes):
        self.P.op("dve", lambda e: e.tensor_reduce(out=out, in_=in_, axis=AX.X, op=op), reads, writes)

    def ld(self, out, in_, key, reads=(), q="sp", slow=False):
        if slow:
            self.P.dma(q, lambda e: e.dma_start(out=out, in_=in_, allow_slow_non_contiguous=True),
                       "L" + key, reads, [key])
        else:
            self.P.dma(q, lambda e: e.dma_start(out=out, in_=in_), "L" + key, reads, [key])

    def st(self, out, in_, key, writes=(), q="pool"):
        self.P.dma(q, lambda e: e.dma_start(out=out, in_=in_), "S" + key, [key], writes)

    def sb(self, es, name, shape, dt):
        self.uid += 1
        nm = "%s_%d" % (name, self.uid)
        return es.enter_context(self.nc.sbuf_tensor(nm, list(shape), dt)), nm

    def pool(self, es, name, shape, dt, n, psum=False):
        self.uid += 1
        return TPool(self, es, "%s_%d_" % (name, self.uid), shape, dt, n, psum)

    def rstd_from_ss(self, ss, key, inv_n):
        self.ts("dve", ss, ss, inv_n, EPS, ALU.mult, ALU.add, [key], [key])
        self.act(ss, ss, AF.Sqrt, [key], [key])
        self.recip(ss, ss, [key], [key])

    def build(self):
        nc = self.nc
        S, L, NT = self.S, self.L, self.NT
        def dr(n, s, dt=F32):
            if n in self.tiny:
                s = [1] * (len(s) - 2) + [128, 128]
            return nc.dram_tensor(n, list(s), dt, kind="ExternalInput").ap()
        sc = lambda n, s, dt=F32: nc.dram_tensor(n, list(s), dt, kind="Internal").ap()
        I = self.I = {}
        I["x"] = dr("x", [S, D])
        I["ctx"] = dr("ctx", [L, D])
        I["ccT"] = dr("ccT", [128, KC, 2])
        I["ada_w"] = dr("ada_w", [2, D, 6 * D])
        I["ada_b"] = dr("ada_b", [2, 6 * D])
        I["w_in"] = dr("w_in", [2, D, DIN])
        I["mg_w"] = dr("mg_w", [2, 4, D, D])
        I["br_w"] = dr("br_w", [2, 4, 512, D])
        I["out_w"] = dr("out_w", [2, D, D])
        I["ffn_w1"] = dr("ffn_w1", [1, D, DFF])
        I["ffn_w3"] = dr("ffn_w3", [1, D, DFF])
        I["ffn_w2"] = dr("ffn_w2", [1, DFF, D])
        I["router"] = dr("router", [128, KC, NE])
        I["moe_w1"] = dr("moe_w1", [1, NE, D, DFE])
        I["moe_w3"] = dr("moe_w3", [1, NE, D, DFE])
        I["moe_w2"] = dr("moe_w2", [1, NE, DFE, D])
        I["n1g"] = dr("n1g", [2, D])
        I["n2g"] = dr("n2g", [2, D])
        I["mcv"] = dr("mcv", [2, 8, 128, 4])
        I["mgb"] = dr("mgb", [2, 64, 16])
        I["mng"] = dr("mng", [2, 512])
        I["cdw"] = dr("cdw", [2, 4, 128, 34])
        I["gqg"] = dr("gqg", [2, 128, 2])
        I["dqg"] = dr("dqg", [2, 64, 2])
        I["dlam"] = dr("dlam", [2, 256])
        I["dsg"] = dr("dsg", [2, 128])
        I["mgbias"] = dr("mgbias", [2, 128, 64])
        I["ident"] = dr("ident", [128, 128])
        I["triF"] = dr("triF", [64, 64])
        I["triB"] = dr("triB", [64, 64])
        I["eye64"] = dr("eye64", [64, 64])
        I["cosG"] = dr("cosG", [128, S])
        I["sinG"] = dr("sinG", [128, S])
        I["cosD"] = dr("cosD", [64, S])
        I["sinD"] = dr("sinD", [64, S])
        I["swapG"] = dr("swapG", [128, 128])
        I["swapD"] = dr("swapD", [64, 64])
        self.out = nc.dram_tensor("out", [S, D], F32, kind="ExternalOutput").ap()
        X = self.X = {}
        X["modd"] = sc("modd", [2, 2, 6 * D])
        def tiled(name, K_, M, nk, cw):
            ncb = (M + cw - 1) // cw
            nkb = (K_ // 128 + nk - 1) // nk
            return dict(ap=sc(name, [ncb, nkb, 128, nk, cw], BF16), nk=nk, cw=cw)
        X["WF"] = [tiled("WF%d" % l, D, NFM, KC, 512) for l in range(2)]
        X["WT"] = [tiled("WT%d" % l, D, NTM, KC, 512) for l in range(2)]
        X["Wg"] = [[tiled("Wg%d_%d" % (l, j), D, D, KC, 512) for j in range(4)] for l in range(2)]
        X["Wb"] = [[tiled("Wb%d_%d" % (l, j), 512, D, 4, 512) for j in range(4)] for l in range(2)]
        X["Wo"] = [tiled("Wo%d" % l, D, D, KC, 512) for l in range(2)]
        X["F1"] = tiled("F1", D, DFF, KC, 256)
        X["F3"] = tiled("F3", D, DFF, KC, 256)
        X["F2"] = tiled("F2", DFF, D, 8, 512)
        X["M1"] = [tiled("M1_%d" % e, D, DFE, KC, 256) for e in range(NE)]
        X["M3"] = [tiled("M3_%d" % e, D, DFE, KC, 256) for e in range(NE)]
        X["M2"] = [tiled("M2_%d" % e, DFE, D, 8, 512) for e in range(NE)]
        X["zF"] = sc("zF", [NFM, NT])
        X["zT"] = sc("zT", [NT, NTM])
        X["hTs"] = sc("hTs", [KC, 128, NT], BF16)
        X["hm"] = sc("hm", [2, NT, 512])
        X["brT"] = sc("brT", [4, 4, 128, NT], BF16)
        X["xA"] = sc("xA", [NT, D])
        X["xB"] = sc("xB", [NT, D])
        X["gws"] = sc("gws", [NT, NE])
        if self.debug_out:
            self.dbg = {k: nc.dram_tensor("dbg_" + k, list(X[k].shape), X[k].dtype, kind="ExternalOutput").ap()
                        for k in self.debug_out}

        with ExitStack() as es:
            self.P = Prog(nc, es)
            self.ident, self.k_ident = self.sb(es, "ident", [128, 128], F32)
            self.ld(self.ident[:], I["ident"][:, :], self.k_ident)
            phases = [("precast", self.phase_precast), ("mod", self.phase_mod)]
            for l in range(self.depth):
                for nm in ("p1", "mlstm", "conf", "attn", "p3a", "p3b"):
                    phases.append(("%s%d" % (nm, l), (lambda l=l, nm=nm: self.run_phase(nm, l))))
            import os
            only = os.environ.get("ONLY")
            for nm, fn in phases:
                if only and nm not in only.split(","):
                    continue
                if only:
                    self.X.setdefault("xcur", self.X["xA"])
                fn()
                if self.stop == nm:
                    break
            if self.debug_out:
                self.phase_debug()
            self.P.emit()
        return nc

    def run_phase(self, nm, l):
        self.l = l
        self.need_ctx = l < self.depth - 1
        getattr(self, "phase_" + nm)(l)

    def groups(self, with_ctx=True, gs=512):
        g = []
        if with_ctx:
            for t0 in range(0, self.L, gs):
                g.append((t0, min(gs, self.L - t0), True))
        for t0 in range(self.L, self.NT, gs):
            g.append((t0, min(gs, self.NT - t0), False))
        return g

    def xsrc(self, l, t0, n):
        if l == 0:
            if t0 < self.L:
                return self.I["ctx"][t0:t0 + n, :]
            return self.I["x"][t0 - self.L:t0 - self.L + n, :]
        return self.X["xcur"][t0:t0 + n, :]

    def phase_debug(self):
        self.P.barrier()
        with ExitStack() as ph:
            pools = {}
            for k in self.debug_out:
                src = self.X[k]
                dst = self.dbg[k]
                if len(src.shape) > 2:
                    src = src.flatten_outer_dims()
                    dst = dst.flatten_outer_dims()
                R, C = src.shape
                dt = src.dtype
                if dt not in pools:
                    pools[dt] = self.pool(ph, "dbgp", [128, 2048], dt, 3)
                for r0 in range(0, R, 128):
                    rn = min(128, R - r0)
                    for c0 in range(0, C, 2048):
                        cw = min(2048, C - c0)
                        t, kt = pools[dt].get()
                        self.ld(t[0:rn, 0:cw], src[r0:r0 + rn, c0:c0 + cw], kt)
                        self.st(dst[r0:r0 + rn, c0:c0 + cw], t[0:rn, 0:cw], kt, q="sp")
            self.P.barrier()

    def cast_mat(self, pools, src, T, col_off=0):
        fpool, bpool = pools
        R, C = src.shape
        nk, cw_t, ap = T["nk"], T["cw"], T["ap"]
        for r0 in range(0, R, 128):
            kca = r0 // 128
            kb, kc = kca // nk, kca % nk
            for c0 in range(0, C, 2048):
                cw = min(2048, C - c0)
                ft, fk = fpool.get()
                bt, bk = bpool.get()
                self.ld(ft[:, 0:cw], src[r0:r0 + 128, c0:c0 + cw], fk)
                eng = ("dve", "act", "pool")[self.rr % 3]
                self.rr += 1
                self.cp(eng, bt[:, 0:cw], ft[:, 0:cw], [fk], [bk])
                a = col_off + c0
                b = a + cw
                x = a
                while x < b:
                    cb = x // cw_t
                    hi = min(b, (cb + 1) * cw_t)
                    self.st(ap[cb, kb, :, kc, x - cb * cw_t:hi - cb * cw_t], bt[:, x - a:hi - a], bk,
                            q="sp" if self.rr % 2 else "pool")
                    x = hi

    def phase_precast(self):
        I, X = self.I, self.X
        self.rr = 0
        with ExitStack() as ph:
            pools = (self.pool(ph, "cf", [128, 2048], F32, 4), self.pool(ph, "cb", [128, 2048], BF16, 4))
            fm_cols = [(0, 1024), (2064, 3088), (3088, 3600), (3600, 3856), (4112, 4624), (4624, 5136), (2048, 2064)]
            tm_cols = [(1024, 1536), (1536, 2048), (3856, 4112), (5136, 5648)]
            for l in range(self.depth):
                o = 0
                for (a, b) in fm_cols:
                    self.cast_mat(pools, I["w_in"][l, :, a:b], X["WF"][l], o)
                    o += b - a
                o = 0
                for (a, b) in tm_cols:
                    self.cast_mat(pools, I["w_in"][l, :, a:b], X["WT"][l], o)
                    o += b - a
                for j in range(4):
                    if "mg_w" not in self.tiny:
                        self.cast_mat(pools, I["mg_w"][l, j], X["Wg"][l][j])
                        self.cast_mat(pools, I["br_w"][l, j], X["Wb"][l][j])
                if "mg_w" not in self.tiny:
                    self.cast_mat(pools, I["out_w"][l], X["Wo"][l])
            if "ffn_w1" not in self.tiny:
                self.cast_mat(pools, I["ffn_w1"][0], X["F1"])
                self.cast_mat(pools, I["ffn_w3"][0], X["F3"])
                self.cast_mat(pools, I["ffn_w2"][0], X["F2"])
            if self.depth > 1 and "moe_w1" not in self.tiny:
                for e in range(NE):
                    self.cast_mat(pools, I["moe_w1"][0, e], X["M1"][e])
                    self.cast_mat(pools, I["moe_w3"][0, e], X["M3"][e])
                    self.cast_mat(pools, I["moe_w2"][0, e], X["M2"][e])
            self.P.barrier()

    def phase_mod(self):
        I, X = self.I, self.X
        with ExitStack() as ph:
            cc, kcc = self.sb(ph, "cc", [128, KC, 2], F32)
            sl, ksl = self.sb(ph, "sl", [128, KC, 2], F32)
            self.ld(cc[:], I["ccT"][:, :, :], kcc)
            self.act(sl[:], cc[:], AF.Silu, [kcc], [ksl])
            wp = self.pool(ph, "aw", [128, KC, 512], F32, 2)
            pp = self.pool(ph, "ap", [128, 512], F32, 2, psum=True)
            bt, kb = self.sb(ph, "ab", [2, 6 * D], F32)
            mr, kmr = self.sb(ph, "mr", [2, 6 * D], F32)
            for l in range(self.depth):
                self.ld(bt[:], I["ada_b"][l, :].partition_broadcast(2), kb)
                for cb in range(6 * D // 512):
                    wt, kw = wp.get()
                    self.ld(wt[:], I["ada_w"][l, :, cb * 512:(cb + 1) * 512].rearrange("(kc p) c -> p kc c", p=128), kw)
                    ps, kp = pp.get()
                    for kc in range(KC):
                        self.mm(ps[0:2, :], sl[:, kc, :], wt[:, kc, :], kc == 0, kc == KC - 1, [ksl, kw], [kp])
                    self.tt("dve", mr[:, cb * 512:(cb + 1) * 512], ps[0:2, :], bt[:, cb * 512:(cb + 1) * 512],
                            ALU.add, [kp, kb], [kmr])
                self.st(X["modd"][l], mr[:], kmr, writes=["modd"])
            self.P.barrier()

    def load_vec(self, tile, key, l, which, idx, normg=None):
        src = self.X["modd"][l, which, idx * D:(idx + 1) * D]
        self.ld(tile[:], src.partition_broadcast(128), key)
        if normg is not None:
            ng, kng = normg
            self.stt("pool", tile[:], tile[:], 1.0, ng[:], ALU.add, ALU.mult, [key, kng], [key])

    def norm_group(self, xsrc_fn, t0, n, G, kG, SH, kSH, xp, hp, sp_, tp, hT, khT, hTf=None):
        ntile = n // 128
        for ti in range(ntile):
            xt, kx = xp.get()
            self.ld(xt[:], xsrc_fn(t0 + ti * 128, 128), kx)
            hf, kh = hp.get()
            ss, ks = sp_.get()
            self.memset("pool", ss[:], 0.0, [ks])
            self.act(hf[:], xt[:], AF.Square, [kx, ks], [kh, ks], accum_out=ss[:])
            self.rstd_from_ss(ss[:], ks, 1.0 / D)
            self.stt("dve", hf[:], xt[:], ss[:, 0:1], G[:], ALU.mult, ALU.mult, [kx, ks, kG], [kh])
            self.tt("pool", hf[:], hf[:], SH[:], ALU.add, [kh, kSH], [kh])
            for q4 in range(KC // 4):
                ps, kp = tp.get()
                for j in range(4):
                    kc = q4 * 4 + j
                    self.tr(ps[:, j * 128:(j + 1) * 128], hf[:, kc * 128:(kc + 1) * 128], self.ident[:],
                            [kh, self.k_ident], [kp])
                dst = hT[:, q4 * 4:(q4 + 1) * 4, ti * 128:(ti + 1) * 128]
                src = ps[:].rearrange("p (j t) -> p j t", j=4)
                if hTf is None:
                    self.cp("act" if q4 % 2 else "dve", dst, src, [kp], [khT])
                else:
                    fdst = hTf[0][:, q4 * 4:(q4 + 1) * 4, ti * 128:(ti + 1) * 128]
                    self.cp("act" if q4 % 2 else "dve", fdst, src, [kp], [hTf[1]])
                    self.cp("pool", dst, fdst, [hTf[1]], [khT])

    def wtile(self, wp, T, k0, nk, c0, ncols):
        wt, kw = wp.get()
        cb = c0 // T["cw"]
        kb = k0 // T["nk"]
        assert c0 % T["cw"] == 0 and k0 % T["nk"] == 0
        self.ld(wt[:, 0:T["nk"], 0:T["cw"]], T["ap"][cb, kb], kw)
        return wt, kw

    def phase_p1(self, l):
        I, X = self.I, self.X
        with ExitStack() as ph:
            ng, kng = self.sb(ph, "ng", [128, D], F32)
            self.ld(ng[:], I["n1g"][l, :].partition_broadcast(128), kng)
            vec = {}
            for which in (0, 1):
                G, kG = self.sb(ph, "G", [128, D], F32)
                SH, kSH = self.sb(ph, "SH", [128, D], F32)
                self.load_vec(G, kG, l, which, 1, (ng, kng))
                self.load_vec(SH, kSH, l, which, 0)
                vec[which] = (G, kG, SH, kSH)
            xp = self.pool(ph, "x", [128, D], F32, 2)
            hp = self.pool(ph, "h", [128, D], F32, 2)
            sp_ = self.pool(ph, "ss", [128, 1], F32, 4)
            tp = self.pool(ph, "tp", [128, 512], F32, 2, psum=True)
            pp = self.pool(ph, "pp", [128, 512], F32, 4, psum=True)
            hTp = self.pool(ph, "hT", [128, KC, 512], BF16, 2)
            wp = self.pool(ph, "w", [128, KC, 512], BF16, 3)
            op_ = self.pool(ph, "o", [128, 512], F32, 4)
            for (t0, n, isctx) in self.groups(True):
                G, kG, SH, kSH = vec[1 if isctx else 0]
                hT, khT = hTp.get()
                self.norm_group(lambda a, b: self.xsrc(l, a, b), t0, n, G, kG, SH, kSH, xp, hp, sp_, tp, hT, khT)
                self.st(X["hTs"][:, :, t0:t0 + n].rearrange("kc p t -> p kc t"), hT[:, :, 0:n], khT,
                        writes=["hTs%d" % t0])
                for c0 in range(0, NFM, 512):
                    cw = min(512, NFM - c0)
                    wt, kw = self.wtile(wp, X["WF"][l], 0, KC, c0, cw)
                    for s0 in range(0, cw, 128):
                        sw = min(128, cw - s0)
                        ps, kp = pp.get()
                        for kc in range(KC):
                            self.mm(ps[0:sw, 0:n], wt[:, kc, s0:s0 + sw], hT[:, kc, 0:n], kc == 0, kc == KC - 1,
                                    [kw, khT], [kp])
                        ot, ko = op_.get()
                        self.cp("act" if (s0 // 128) % 2 else "dve", ot[0:sw, 0:n], ps[0:sw, 0:n], [kp], [ko])
                        self.st(X["zF"][c0 + s0:c0 + s0 + sw, t0:t0 + n], ot[0:sw, 0:n], ko, writes=["zF%d" % t0])
                for c0 in range(0, NTM, 512):
                    cw = min(512, NTM - c0)
                    wt, kw = self.wtile(wp, X["WT"][l], 0, KC, c0, cw)
                    for ti in range(n // 128):
                        ps, kp = pp.get()
                        for kc in range(KC):
                            self.mm(ps[:, 0:cw], hT[:, kc, ti * 128:(ti + 1) * 128], wt[:, kc, 0:cw], kc == 0,
                                    kc == KC - 1, [kw, khT], [kp])
                        ot, ko = op_.get()
                        self.cp("act" if ti % 2 else "dve", ot[:, 0:cw], ps[:, 0:cw], [kp], [ko])
                        self.st(X["zT"][t0 + ti * 128:t0 + (ti + 1) * 128, c0:c0 + cw], ot[:, 0:cw], ko,
                                writes=["zT%d" % t0])
            self.P.barrier()

    def phase_mlstm(self, l):
        I, X = self.I, self.X
        S, L, NT = self.S, self.L, self.NT
        with ExitStack() as ph:
            triF, ktF = self.sb(ph, "triF", [64, 64], F32)
            triB, ktB = self.sb(ph, "triB", [64, 64], F32)
            eye, keye = self.sb(ph, "eye", [64, 64], F32)
            ones, kones = self.sb(ph, "ones", [64, 128], F32)
            mgb, kmgb = self.sb(ph, "mgb", [64, 16], F32)
            nmgb, knmgb = self.sb(ph, "nmgb", [64, 16], F32)
            mcv, kmcv = self.sb(ph, "mcv", [128, 8, 4], F32)
            lnsc, klnsc = self.sb(ph, "lnsc", [128, 1], F32)
            self.ld(triF[:], I["triF"][:, :], ktF)
            self.ld(triB[:], I["triB"][:, :], ktB)
            self.ld(eye[:], I["eye64"][:, :], keye)
            self.ld(mgb[:], I["mgb"][l], kmgb)
            self.ld(mcv[:], I["mcv"][l].rearrange("a p k -> p a k"), kmcv)
            self.memset("dve", ones[:], 1.0, [kones])
            self.memset("dve", lnsc[:], -0.5 * math.log(128.0), [klnsc])
            one1, kone1 = self.sb(ph, "one1", [128, 1], F32)
            self.memset("dve", one1[:], 1.0, [kone1])
            self.ts("dve", nmgb[:], mgb[:], -1.0, None, ALU.mult, None, [kmgb], [knmgb])
            NB = 16
            rawp = self.pool(ph, "raw", [128, NB * 64 + 2], F32, 2)
            accp = self.pool(ph, "acc", [128, NB * 64], F32, 2)
            qcp = self.pool(ph, "qc", [128, NB * 64], F32, 2)
            gp = self.pool(ph, "g", [64, 2, NB], F32, 2)
            spp = self.pool(ph, "spn", [64, 2, NB], F32, 2)
            x1p = self.pool(ph, "x1", [64, NB, 64], F32, 2)
            x2p = self.pool(ph, "x2", [64, NB, 64], F32, 2)
            ebp = self.pool(ph, "eb", [128, NB * 64], F32, 2)
            decp = self.pool(ph, "dec", [128, NB], F32, 2)
            qbp = self.pool(ph, "qb", [128, NB * 64], BF16, 2)
            kfp = self.pool(ph, "kf", [128, NB * 64], F32, 2)
            kbp = self.pool(ph, "kb", [128, NB * 64], BF16, 2)
            ktp = self.pool(ph, "kt", [64, NB, 128], BF16, 2)
            vfp = self.pool(ph, "vf", [64, NB, 128], F32, 2)
            vap = self.pool(ph, "va", [64, NB, 129], BF16, 2)
            hop = self.pool(ph, "ho", [64, NB, 128], F32, 2)
            wmp = self.pool(ph, "wm", [64, 64], BF16, 3)
            dnp = self.pool(ph, "dn", [64, 1], F32, 4)
            tmp_, ktmp = self.sb(ph, "stmp", [128, 129], F32)
            Sf, kSf = self.sb(ph, "Sf", [128, 129], F32)
            Sb, kSb = self.sb(ph, "Sb", [128, 129], BF16)
            pp = self.pool(ph, "mp", [128, 512], F32, 6, psum=True)

            segs = [(0, L), (L, NT)]
            for h in range(4):
                for d in range(2):
                    tri, ktri = (triF, ktF) if d == 0 else (triB, ktB)
                    self.memset("dve", Sf[:], 0.0, [kSf])
                    self.memset("pool", Sb[:], 0.0, [kSb])
                    for (s0, s1) in segs:
                        blocks = [(b0, min(NB, (s1 - b0) // 64)) for b0 in range(s0, s1, NB * 64)]
                        if d == 1:
                            blocks = blocks[::-1]
                        for (b0, nb) in blocks:
                            n = nb * 64
                            qk = []
                            for a in range(2):
                                raw, kr = rawp.get()
                                row0 = a * 512 + h * 128
                                lo = max(s0, b0 - 1)
                                hi = min(s1, b0 + n + 1)
                                if lo > b0 - 1:
                                    self.memset("pool", raw[:, 0:1], 0.0, [kr])
                                if hi < b0 + n + 1:
                                    self.memset("pool", raw[:, n + 1:n + 2], 0.0, [kr])
                                self.ld(raw[:, lo - (b0 - 1):hi - (b0 - 1)], X["zF"][row0:row0 + 128, lo:hi], kr,
                                        reads=["zF%d" % g[0] for g in self.groups(True)])
                                acc, ka = accp.get()
                                ci = a * 4 + h
                                self.ts("dve", acc[:, 0:n], raw[:, 0:n], mcv[:, ci, 0:1], None, ALU.mult, None,
                                        [kr, kmcv], [ka])
                                self.stt("dve", acc[:, 0:n], raw[:, 1:n + 1], mcv[:, ci, 1:2], acc[:, 0:n], ALU.mult,
                                         ALU.add, [kr, kmcv, ka], [ka])
                                self.stt("dve", acc[:, 0:n], raw[:, 2:n + 2], mcv[:, ci, 2:3], acc[:, 0:n], ALU.mult,
                                         ALU.add, [kr, kmcv, ka], [ka])
                                qc, kq = qcp.get()
                                self.act(qc[:, 0:n], acc[:, 0:n], AF.Silu, [ka, kmcv], [kq], bias=mcv[:, ci, 3:4])
                                qk.append((qc, kq))
                            g, kg = gp.get()
                            zr = 3840 + d * 8 + h
                            for a in range(2):
                                self.ld(g[:, a, 0:nb], X["zF"][zr + 4 * a, b0:b0 + n].rearrange("(c u) -> u c", u=64), kg,
                                        reads=["zF%d" % gg[0] for gg in self.groups(True)], slow=True)
                            spn, ks = spp.get()
                            bi = d * 8 + h
                            self.ts("dve", spn[:, 0, 0:nb], g[:, 0, 0:nb], mgb[:, bi:bi + 1], None, ALU.add, None,
                                    [kg, kmgb], [ks])
                            self.act(spn[:, 1, 0:nb], g[:, 1, 0:nb], AF.Exp, [kg, knmgb], [ks], scale=-1.0,
                                     bias=nmgb[:, bi + 4:bi + 5])
                            self.act(spn[:, 1, 0:nb], spn[:, 1, 0:nb], AF.Ln, [ks, kone1], [ks], bias=one1[0:64, 0:1])
                            self.ts("dve", spn[:, 1, 0:nb], spn[:, 1, 0:nb], -1.0, None, ALU.mult, None, [ks], [ks])
                            x1, k1 = x1p.get()
                            x2, k2 = x2p.get()
                            lf_b = spn[:, 1, 0:nb].unsqueeze(2).broadcast_to([64, nb, 64])
                            i_b = spn[:, 0, 0:nb].unsqueeze(2).broadcast_to([64, nb, 64])
                            tri_b = tri[:].unsqueeze(1).broadcast_to([64, nb, 64])
                            eye_b = eye[:].unsqueeze(1).broadcast_to([64, nb, 64])
                            self.tt("dve", x1[:, 0:nb, :], lf_b, tri_b, ALU.mult, [ks, ktri], [k1])
                            self.tt("pool", x2[:, 0:nb, :], i_b, eye_b, ALU.mult, [ks, keye], [k2])
                            self.tt("pool", x2[:, 0:nb, :], x2[:, 0:nb, :], x1[:, 0:nb, :], ALU.subtract, [k1, k2], [k2])
                            eq, keq = ebp.get()
                            ek, kek = ebp.get()
                            x1f = x1[:].rearrange("u c t -> u (c t)")
                            x2f = x2[:].rearrange("u c t -> u (c t)")
                            for c0 in range(0, n, 512):
                                cw = min(512, n - c0)
                                ps, kp = pp.get()
                                self.mm(ps[:, 0:cw], ones[:], x1f[:, c0:c0 + cw], True, True, [kones, k1], [kp])
                                self.act(eq[:, c0:c0 + cw], ps[:, 0:cw], AF.Exp, [kp], [keq])
                                ps, kp = pp.get()
                                self.mm(ps[:, 0:cw], ones[:], x2f[:, c0:c0 + cw], True, True, [kones, k2], [kp])
                                self.act(ek[:, c0:c0 + cw], ps[:, 0:cw], AF.Exp, [kp, klnsc], [kek], bias=lnsc[:, 0:1])
                            dec, kdec = decp.get()
                            ps, kp = pp.get()
                            self.mm(ps[:, 0:nb], ones[:], spn[:, 1, 0:nb], True, True, [kones, ks], [kp])
                            self.act(dec[:, 0:nb], ps[:, 0:nb], AF.Exp, [kp], [kdec])
                            qb, kqb = qbp.get()
                            kf, kkf = kfp.get()
                            kb, kkb = kbp.get()
                            self.tt("dve", qb[:, 0:n], qk[0][0][:, 0:n], eq[:, 0:n], ALU.mult, [qk[0][1], keq], [kqb])
                            self.tt("pool", kf[:, 0:n], qk[1][0][:, 0:n], ek[:, 0:n], ALU.mult, [qk[1][1], kek], [kkf])
                            self.cp("act", kb[:, 0:n], kf[:, 0:n], [kkf], [kkb])
                            kt, kkt = ktp.get()
                            for c4 in range(0, nb, 4):
                                cn = min(4, nb - c4)
                                ps, kp = pp.get()
                                for j in range(cn):
                                    c = c4 + j
                                    self.tr(ps[0:64, j * 128:(j + 1) * 128], kf[:, c * 64:(c + 1) * 64], self.ident[:],
                                            [kkf, self.k_ident], [kp])
                                self.cp("act", kt[:, c4:c4 + cn, :],
                                        ps[0:64, 0:cn * 128].rearrange("s (j d) -> s j d", j=cn), [kp], [kkt])
                            vf, kvf = vfp.get()
                            va, kva = vap.get()
                            self.ld(vf[:, 0:nb, :],
                                    X["zT"][b0:b0 + n, h * 128:(h + 1) * 128].rearrange("(c s) d -> s c d", s=64), kvf,
                                    reads=["zT%d" % gg[0] for gg in self.groups(True)])
                            self.cp("pool", va[:, 0:nb, 0:128], vf[:, 0:nb, :], [kvf], [kva])
                            self.memset("pool", va[:, 0:nb, 128:129], 1.0, [kva])
                            ho, kho = hop.get()
                            order = list(range(nb)) if d == 0 else list(range(nb))[::-1]
                            for c in order:
                                cs = slice(c * 64, (c + 1) * 64)
                                psU, kU = pp.get()
                                self.mm(psU[:, 0:129], kt[:, c, :], va[:, c, :], True, True, [kkt, kva], [kU])
                                psW, kW = pp.get()
                                self.mm(psW[0:64, 0:64], kb[:, cs], qb[:, cs], True, True, [kkb, kqb], [kW])
                                wm, kwm = wmp.get()
                                self.tt("dve", wm[:], psW[0:64, 0:64], tri[:], ALU.mult, [kW, ktri], [kwm])
                                psN, kN = pp.get()
                                self.mm(psN[0:64, 0:129], qb[:, cs], Sb[:], True, False, [kqb, kSb], [kN])
                                self.mm(psN[0:64, 0:129], wm[:], va[:, c, :], False, True, [kwm, kva], [kN])
                                dn, kdn = dnp.get()
                                self.act(dn[:], psN[0:64, 128:129], AF.Abs, [kN], [kdn])
                                self.ts("dve", dn[:], dn[:], 1.0, None, ALU.max, None, [kdn], [kdn])
                                self.recip(dn[:], dn[:], [kdn], [kdn])
                                self.ts("dve", ho[:, c, :], psN[0:64, 0:128], dn[:, 0:1], None, ALU.mult, None,
                                        [kN, kdn], [kho])
                                self.tt("dve", tmp_[:], psU[:, 0:129], Sf[:], ALU.add, [kU, kSf], [ktmp])
                                self.ts("dve", Sf[:], tmp_[:], dec[:, c:c + 1], None, ALU.mult, None, [ktmp, kdec], [kSf])
                                self.cp("act", Sb[:], Sf[:], [kSf], [kSb])
                            self.st(X["hm"][d, b0:b0 + n, h * 128:(h + 1) * 128].rearrange("(c s) e -> s c e", s=64),
                                    ho[:, 0:nb, :], kho, writes=["hm"])
            self.P.barrier()
            with ExitStack() as ph2:
                gm, kgm = self.sb(ph2, "gm", [128, 512], F32)
                self.ld(gm[:], I["mng"][l, :].partition_broadcast(128), kgm)
                hp = self.pool(ph2, "hh", [128, 3, 512], F32, 2)
                sqp = self.pool(ph2, "sq", [128, 512], F32, 2)
                ssp = self.pool(ph2, "ss4", [128, 4], F32, 2)
                ap_ = self.pool(ph2, "a", [128, 512], F32, 2)
                abp = self.pool(ph2, "ab", [128, 4, 128], BF16, 2)
                tp = self.pool(ph2, "tp", [128, 512], F32, 2, psum=True)
                t_lo = 0 if self.need_ctx else L
                for t0 in range(t_lo, NT, 128):
                    hh, kh = hp.get()
                    self.ld(hh[:, 0, :], X["hm"][0, t0:t0 + 128, :], kh)
                    self.ld(hh[:, 1, :], X["hm"][1, t0:t0 + 128, :], kh)
                    self.ld(hh[:, 2, :], X["zT"][t0:t0 + 128, 512:1024], kh)
                    a, ka = ap_.get()
                    self.tt("dve", a[:], hh[:, 0, :], hh[:, 1, :], ALU.add, [kh], [ka])
                    sq, ksq = sqp.get()
                    self.act(sq[:], a[:], AF.Square, [ka], [ksq])
                    ss, kss = ssp.get()
                    self.red(ss[:], sq[:].rearrange("p (h e) -> p h e", h=4), ALU.add, [ksq], [kss])
                    self.rstd_from_ss(ss[:], kss, 1.0 / 128)
                    self.tt("dve", a[:].rearrange("p (h e) -> p h e", h=4), a[:].rearrange("p (h e) -> p h e", h=4),
                            ss[:].unsqueeze(2).broadcast_to([128, 4, 128]), ALU.mult, [ka, kss], [ka])
                    self.tt("pool", a[:], a[:], gm[:], ALU.mult, [ka, kgm], [ka])
                    self.act(sq[:], hh[:, 2, :], AF.Sigmoid, [kh], [ksq])
                    self.tt("dve", a[:], a[:], sq[:], ALU.mult, [ka, ksq], [ka])
                    self.to_brT(0, a, ka, t0, tp, abp)
                self.P.barrier()

    def to_brT(self, j, a, ka, t0, tp, abp):
        ps, kp = tp.get()
        for kc in range(4):
            self.tr(ps[:, kc * 128:(kc + 1) * 128], a[:, kc * 128:(kc + 1) * 128], self.ident[:], [ka, self.k_ident],
                    [kp])
        ab, kab = abp.get()
        self.cp("act", ab[:], ps[:].rearrange("p (k t) -> p k t", k=4), [kp], [kab])
        self.st(self.X["brT"][j, :, :, t0:t0 + 128].rearrange("k p t -> p k t"), ab[:], kab, writes=["brT"])

    def phase_conf(self, l):
        I, X = self.I, self.X
        S, L, NT = self.S, self.L, self.NT
        with ExitStack() as ph:
            cdw, kcdw = self.sb(ph, "cdw", [128, 4, 34], F32)
            self.ld(cdw[:], I["cdw"][l].rearrange("c p k -> p c k"), kcdw)
            onesm, kom = self.sb(ph, "onesm", [128, 128], F32)
            self.memset("dve", onesm[:], 1.0 / 512, [kom])
            inp = self.pool(ph, "cin", [128, 2, 542], F32, 3)
            up = self.pool(ph, "cu", [128, 542], F32, 2)
            a0p = self.pool(ph, "ca0", [128, 512], F32, 2)
            a1p = self.pool(ph, "ca1", [128, 512], F32, 2)
            cc, kcc = self.sb(ph, "ccv", [128, 4, 512], F32)
            sq, ksq = self.sb(ph, "csq", [128, 4, 512], F32)
            rs, krs = self.sb(ph, "crs", [128, 512], F32)
            obp = self.pool(ph, "cob", [128, 4, 512], BF16, 2)
            pp = self.pool(ph, "cp", [128, 512], F32, 2, psum=True)
            segs = ([(0, L)] if self.need_ctx else []) + [(L, NT)]
            zdeps = ["zF%d" % g[0] for g in self.groups(True)]
            for (s0, s1) in segs:
                for t0 in range(s0, s1, 512):
                    n = min(512, s1 - t0)
                    lo, hi = max(s0, t0 - 15), min(s1, t0 + n + 15)
                    for ct in range(4):
                        it, ki = inp.get()
                        if lo > t0 - 15:
                            self.memset("pool", it[:, :, 0:15], 0.0, [ki])
                        if hi < t0 + n + 15:
                            self.memset("pool", it[:, :, n + 15:n + 30], 0.0, [ki])
                        for a in range(2):
                            r0 = 1024 + a * 512 + ct * 128
                            self.ld(it[:, a, lo - (t0 - 15):hi - (t0 - 15)], X["zF"][r0:r0 + 128, lo:hi], ki, reads=zdeps)
                        u, ku = up.get()
                        self.act(u[:, 0:n + 30], it[:, 1, 0:n + 30], AF.Sigmoid, [ki], [ku])
                        self.tt("dve", u[:, 0:n + 30], u[:, 0:n + 30], it[:, 0, 0:n + 30], ALU.mult, [ku, ki], [ku])
                        a0, k0 = a0p.get()
                        a1, k1 = a1p.get()
                        self.ts("dve", a0[:, 0:n], u[:, 0:n], cdw[:, ct, 0:1], cdw[:, ct, 31:32], ALU.mult, ALU.add,
                                [ku, kcdw], [k0])
                        self.ts("pool", a1[:, 0:n], u[:, 1:n + 1], cdw[:, ct, 1:2], None, ALU.mult, None, [ku, kcdw], [k1])
                        for k in range(2, 31):
                            if k % 2 == 0:
                                self.stt("dve", a0[:, 0:n], u[:, k:k + n], cdw[:, ct, k:k + 1], a0[:, 0:n], ALU.mult,
                                         ALU.add, [ku, kcdw, k0], [k0])
                            else:
                                self.stt("pool", a1[:, 0:n], u[:, k:k + n], cdw[:, ct, k:k + 1], a1[:, 0:n], ALU.mult,
                                         ALU.add, [ku, kcdw, k1], [k1])
                        self.tt("dve", cc[:, ct, 0:n], a0[:, 0:n], a1[:, 0:n], ALU.add, [k0, k1], [kcc])
                    ps, kp = pp.get()
                    for ct in range(4):
                        self.mm(ps[:, 0:n], onesm[:], cc[:, ct, 0:n], ct == 0, ct == 3, [kom, kcc], [kp])
                    for ct in range(4):
                        self.tt("dve", cc[:, ct, 0:n], cc[:, ct, 0:n], ps[:, 0:n], ALU.subtract, [kcc, kp], [kcc])
                    self.act(sq[:, :, 0:n], cc[:, :, 0:n], AF.Square, [kcc], [ksq])
                    ps, kp = pp.get()
                    for ct in range(4):
                        self.mm(ps[:, 0:n], onesm[:], sq[:, ct, 0:n], ct == 0, ct == 3, [kom, ksq], [kp])
                    self.ts("dve", rs[:, 0:n], ps[:, 0:n], EPS, None, ALU.add, None, [kp], [krs])
                    self.act(rs[:, 0:n], rs[:, 0:n], AF.Sqrt, [krs], [krs])
                    self.recip(rs[:, 0:n], rs[:, 0:n], [krs], [krs])
                    ob, kob = obp.get()
                    for ct in range(4):
                        self.tt("dve", cc[:, ct, 0:n], cc[:, ct, 0:n], rs[:, 0:n], ALU.mult, [kcc, krs], [kcc])
                        self.act(ob[:, ct, 0:n], cc[:, ct, 0:n], AF.Silu, [kcc, kcdw], [kob], scale=cdw[:, ct, 32:33],
                                 bias=cdw[:, ct, 33:34])
                    self.st(X["brT"][1, :, :, t0:t0 + n].rearrange("k p t -> p k t"), ob[:, :, 0:n], kob, writes=["brT"])
            self.P.barrier()

    def prep_qk(self, d, src_rows, t0, n, g_ap, kg, rope_off, tabs, pools, dst, kdst, dst_off):
        X = self.X
        rawp, sqp, pp, onesd, kod, swp, ksw = pools
        raw, kr = rawp.get()
        self.ld(raw[0:d, 0:n], X["zF"][src_rows:src_rows + d, t0:t0 + n], kr,
                reads=["zF%d" % g[0] for g in self.groups(True)])
        sq, ksq = sqp.get()
        self.act(sq[0:d, 0:n], raw[0:d, 0:n], AF.Square, [kr], [ksq])
        ps, kp = pp.get()
        self.mm(ps[0:d, 0:n], onesd[0:d, 0:d], sq[0:d, 0:n], True, True, [kod, ksq], [kp])
        self.ts("dve", sq[0:d, 0:n], ps[0:d, 0:n], EPS, None, ALU.add, None, [kp], [ksq])
        self.act(sq[0:d, 0:n], sq[0:d, 0:n], AF.Sqrt, [ksq], [ksq])
        self.recip(sq[0:d, 0:n], sq[0:d, 0:n], [ksq], [ksq])
        if rope_off is None:
            self.stt("dve", dst[0:d, dst_off:dst_off + n], raw[0:d, 0:n], g_ap, sq[0:d, 0:n], ALU.mult, ALU.mult,
                     [kr, kg, ksq], [kdst])
            return
        self.stt("dve", raw[0:d, 0:n], raw[0:d, 0:n], g_ap, sq[0:d, 0:n], ALU.mult, ALU.mult, [kr, kg, ksq], [kr])
        ps, kp = pp.get()
        self.mm(ps[0:d, 0:n], swp[0:d, 0:d], raw[0:d, 0:n], True, True, [ksw, kr], [kp])
        cosD_, sinD_, tabp = tabs
        tb, kct = tabp.get()
        self.ld(tb[0:d, 0, 0:n], cosD_[0:d, rope_off:rope_off + n], kct)
        self.ld(tb[0:d, 1, 0:n], sinD_[0:d, rope_off:rope_off + n], kct)
        self.tt("dve", sq[0:d, 0:n], ps[0:d, 0:n], tb[0:d, 1, 0:n], ALU.mult, [kp, kct], [ksq])
        self.tt("pool", raw[0:d, 0:n], raw[0:d, 0:n], tb[0:d, 0, 0:n], ALU.mult, [kr, kct], [kr])
        self.tt("dve", dst[0:d, dst_off:dst_off + n], raw[0:d, 0:n], sq[0:d, 0:n], ALU.add, [kr, ksq], [kdst])

    def phase_attn(self, l):
        I, X = self.I, self.X
        S, L, NT = self.S, self.L, self.NT
        NKT = NT // 128
        lam_init = 0.8 - 0.6 * math.exp(-0.3 * l)
        with ExitStack() as ph:
            tabp = self.pool(ph, "tab", [128, 2, 512], F32, 2)
            swG, kswG = self.sb(ph, "swG", [128, 128], F32)
            swD, kswD = self.sb(ph, "swD", [64, 64], F32)
            self.ld(swG[:], I["swapG"][:, :], kswG)
            self.ld(swD[:], I["swapD"][:, :], kswD)
            onG, konG = self.sb(ph, "onG", [128, 128], F32)
            onD, konD = self.sb(ph, "onD", [64, 64], F32)
            self.memset("dve", onG[:], 1.0 / 128, [konG])
            self.memset("dve", onD[:], 1.0 / 64, [konD])
            gq, kgq = self.sb(ph, "gq", [128, 2], F32)
            dq, kdq = self.sb(ph, "dq", [64, 2], F32)
            self.ld(gq[:], I["gqg"][l], kgq)
            self.ld(dq[:], I["dqg"][l], kdq)
            dl, kdl = self.sb(ph, "dl", [128, 4, 64], F32)
            self.ld(dl[:].rearrange("p a e -> p (a e)"), I["dlam"][l, :].partition_broadcast(128), kdl)
            lam, klam = self.sb(ph, "lam", [128, 4], F32)
            pr, kpr = self.sb(ph, "pr", [128, 2, 64], F32)
            self.tt("dve", pr[:, 0, :], dl[:, 0, :], dl[:, 1, :], ALU.mult, [kdl], [kpr])
            self.tt("dve", pr[:, 1, :], dl[:, 2, :], dl[:, 3, :], ALU.mult, [kdl], [kpr])
            self.red(lam[:, 0:2], pr[:], ALU.add, [kpr], [klam])
            self.act(lam[:, 0:2], lam[:, 0:2], AF.Exp, [klam], [klam])
            self.tt("dve", lam[:, 2:3], lam[:, 1:2], lam[:, 0:1], ALU.subtract, [klam], [klam])
            self.ts("dve", lam[:, 2:3], lam[:, 2:3], -lam_init, None, ALU.add, None, [klam], [klam])
            sg, ksg = self.sb(ph, "sg", [128, 128], F32)
            self.ld(sg[:], I["dsg"][l, :].partition_broadcast(128), ksg)

            rawp = self.pool(ph, "araw", [128, 512], F32, 2)
            sqp = self.pool(ph, "asq", [128, 512], F32, 2)
            pp = self.pool(ph, "app", [128, 512], F32, 2, psum=True)
            psS = self.pool(ph, "psS", [128, 512], F32, 2, psum=True)
            psO = self.pool(ph, "psO", [128, 512], F32, 4, psum=True)
            KT, kKT = self.sb(ph, "KT", [128, NT], BF16)
            KT2, kKT2 = self.sb(ph, "KT2", [64, NT], BF16)
            vf_p = self.pool(ph, "avf", [128, 128], F32, 3)
            VA, kVA = self.sb(ph, "VA", [128, NKT, 129], BF16)
            QTp = self.pool(ph, "QT", [128, 512], BF16, 2)
            QT2p = self.pool(ph, "QT2", [64, 512], BF16, 2)
            PTp = self.pool(ph, "PT", [128, 512], BF16, 3)
            rdp = self.pool(ph, "rd", [128, 1], F32, 4)
            o1p = self.pool(ph, "o1", [128, 4, 128], F32, 2)
            o2p = self.pool(ph, "o2", [128, 4, 128], F32, 2)
            abp = self.pool(ph, "aab", [128, 512], BF16, 2)
            ssp = self.pool(ph, "ass", [128, 1], F32, 4)
            poolsG = (rawp, sqp, pp, onG, konG, swG, kswG)
            poolsD = (rawp, sqp, pp, onD, konD, swD, kswD)
            tabsG = (I["cosG"], I["sinG"], tabp)
            tabsD = (I["cosD"], I["sinD"], tabp)
            zTdeps = ["zT%d" % g[0] for g in self.groups(True)]

            def load_v(c0):
                for kt in range(NKT):
                    vf, kvf = vf_p.get()
                    self.ld(vf[:], X["zT"][kt * 128:(kt + 1) * 128, c0:c0 + 128], kvf, reads=zTdeps)
                    self.cp("pool", VA[:, kt, 0:128], vf[:], [kvf], [kVA])
                self.memset("pool", VA[:, :, 128:129], 1.0, [kVA])

            def key_ranges():
                r = []
                t = 0
                while t < L:
                    n = min(512, L - t); r.append((t, n)); t += n
                while t < NT:
                    n = min(512, NT - t); r.append((t, n)); t += n
                return r

            def prep_keys2(d, row0, gap, kg, pools, tabs, dst, kdst):
                for (t0, n) in key_ranges():
                    ro = None if t0 < L else t0 - L
                    self.prep_qk(d, row0, t0, n, gap, kg, ro, tabs, pools, dst, kdst, t0)

            def attend(d, QT, kQT, nq, KTt, kKTt, kt_lo, kt_hi, scale):
                accs = [psO.get() for _ in range(nq // 128)]
                for kt in range(kt_lo, kt_hi):
                    ps, kp = psS.get()
                    self.mm(ps[:, 0:nq], KTt[0:d, kt * 128:(kt + 1) * 128], QT[0:d, 0:nq], True, True, [kKTt, kQT], [kp])
                    pt, kpt = PTp.get()
                    self.act(pt[:, 0:nq], ps[:, 0:nq], AF.Exp, [kp], [kpt], scale=scale)
                    for j, (po, kpo) in enumerate(accs):
                        self.mm(po[:, 0:129], pt[:, j * 128:(j + 1) * 128], VA[:, kt, :], kt == kt_lo, kt == kt_hi - 1,
                                [kpt, kVA], [kpo])
                return accs

            def normalize(accs, o, ko):
                for j, (po, kpo) in enumerate(accs):
                    rd, krd = rdp.get()
                    self.recip(rd[:], po[:, 128:129], [kpo], [krd])
                    self.ts("dve", o[:, j, :], po[:, 0:128], rd[:, 0:1], None, ALU.mult, None, [kpo, krd], [ko])

            def qblocks():
                r = []
                if self.need_ctx:
                    t = 0
                    while t < L:
                        n = min(512, L - t); r.append((t, n, True)); t += n
                t = L
                while t < NT:
                    n = min(512, NT - t); r.append((t, n, False)); t += n
                return r

            def emit_T(j, head, o, ko, t0, nq):
                ps, kp = pp.get()
                for i in range(nq // 128):
                    self.tr(ps[:, i * 128:(i + 1) * 128], o[:, i, :], self.ident[:], [ko, self.k_ident], [kp])
                ab, kab = abp.get()
                self.cp("act", ab[:, 0:nq], ps[:, 0:nq], [kp], [kab])
                self.st(X["brT"][j, head, :, t0:t0 + nq], ab[:, 0:nq], kab, writes=["brT"])

            for kvh in range(2):
                prep_keys2(128, 2560 + kvh * 128, gq[:, 1:2], kgq, poolsG, tabsG, KT, kKT)
                load_v(1024 + kvh * 128)
                for hq in range(2):
                    head = kvh * 2 + hq
                    for (t0, nq, isctx) in qblocks():
                        QT, kQT = QTp.get()
                        self.prep_qk(128, 2048 + head * 128, t0, nq, gq[:, 0:1], kgq, None if isctx else t0 - L, tabsG,
                                     poolsG, QT, kQT, 0)
                        accs = attend(128, QT, kQT, nq, KT, kKT, 0, (L // 128) if isctx else NKT, 128 ** -0.5)
                        o, ko = o1p.get()
                        normalize(accs, o, ko)
                        emit_T(2, head, o, ko, t0, nq)
            for head in range(4):
                prep_keys2(64, 3328 + head * 128, dq[:, 1:2], kdq, poolsD, tabsD, KT, kKT)
                prep_keys2(64, 3328 + head * 128 + 64, dq[:, 1:2], kdq, poolsD, tabsD, KT2, kKT2)
                load_v(1280 + head * 128)
                for (t0, nq, isctx) in qblocks():
                    ro = None if isctx else t0 - L
                    kt_hi = (L // 128) if isctx else NKT
                    QT, kQT = QT2p.get()
                    self.prep_qk(64, 2816 + head * 128, t0, nq, dq[:, 0:1], kdq, ro, tabsD, poolsD, QT, kQT, 0)
                    accs = attend(64, QT, kQT, nq, KT, kKT, 0, kt_hi, 64 ** -0.5)
                    o1, ko1 = o1p.get()
                    normalize(accs, o1, ko1)
                    QT, kQT = QT2p.get()
                    self.prep_qk(64, 2816 + head * 128 + 64, t0, nq, dq[:, 0:1], kdq, ro, tabsD, poolsD, QT, kQT, 0)
                    accs = attend(64, QT, kQT, nq, KT2, kKT2, 0, kt_hi, 64 ** -0.5)
                    o2, ko2 = o2p.get()
                    normalize(accs, o2, ko2)
                    nj = nq // 128
                    self.stt("dve", o1[:, 0:nj, :], o2[:, 0:nj, :], lam[:, 2:3], o1[:, 0:nj, :], ALU.mult, ALU.add,
                             [ko2, klam, ko1], [ko1])
                    for j in range(nj):
                        ss, kss = ssp.get()
                        self.memset("pool", ss[:], 0.0, [kss])
                        self.act(o2[:, j, :], o1[:, j, :], AF.Square, [ko1, kss], [ko2, kss], accum_out=ss[:])
                        self.rstd_from_ss(ss[:], kss, 1.0 / 128)
                        self.stt("dve", o1[:, j, :], o1[:, j, :], ss[:, 0:1], sg[:], ALU.mult, ALU.mult, [ko1, kss, ksg],
                                 [ko1])
                    self.ts("dve", o1[:, 0:nj, :], o1[:, 0:nj, :], 1.0 - lam_init, None, ALU.mult, None, [ko1], [ko1])
                    emit_T(3, head, o1, ko1, t0, nq)
            self.P.barrier()

    def phase_p3a(self, l):
        I, X = self.I, self.X
        L = self.L
        self.X["xnext"] = X["xB"] if X.get("xcur") is X["xA"] else X["xA"]
        with ExitStack() as ph:
            gate = {}
            for which in ((0, 1) if self.need_ctx else (0,)):
                g, kg = self.sb(ph, "g1", [128, D], F32)
                self.load_vec(g, kg, l, which, 2)
                gate[which] = (g, kg)
            mgb, kmgb = self.sb(ph, "mgbias", [128, 4, 16], F32)
            self.ld(mgb[:].rearrange("p j f -> p (j f)"), I["mgbias"][l], kmgb)
            hTp = self.pool(ph, "hT", [128, KC, 512], BF16, 1)
            brp = self.pool(ph, "br", [128, 4, 4, 512], BF16, 1)
            wgp = self.pool(ph, "wg", [128, KC, 512], BF16, 3)
            wbp = self.pool(ph, "wb", [128, 4, 512], BF16, 3)
            yT, kyT = self.sb(ph, "yT", [128, KC, 512], BF16)
            yacc, kya = self.sb(ph, "yacc", [128, 4, 512], F32)
            sgp = self.pool(ph, "sg", [128, 512], F32, 3)
            tmp = self.pool(ph, "tm", [128, 512], F32, 3)
            xp = self.pool(ph, "xx", [128, 512], F32, 3)
            op_ = self.pool(ph, "oo", [128, 512], F32, 3)
            pG = self.pool(ph, "pG", [128, 512], F32, 3, psum=True)
            pB = self.pool(ph, "pB", [128, 512], F32, 3, psum=True)
            pO = self.pool(ph, "pO", [128, 512], F32, 2, psum=True)
            for (t0, n, isctx) in self.groups(self.need_ctx):
                g1, kg1 = gate[1 if isctx else 0]
                hT, khT = hTp.get()
                self.ld(hT[:, :, 0:n], X["hTs"][:, :, t0:t0 + n].rearrange("kc p t -> p kc t"), khT,
                        reads=["hTs%d" % t0])
                br, kbr = brp.get()
                for j in range(4):
                    self.ld(br[:, j, :, 0:n], X["brT"][j, :, :, t0:t0 + n].rearrange("k p t -> p k t"), kbr, reads=["brT"])
                for fb in range(4):
                    for j in range(4):
                        wg, kwg = self.wtile(wgp, X["Wg"][l][j], 0, KC, fb * 512, 512)
                        wb, kwb = self.wtile(wbp, X["Wb"][l][j], 0, 4, fb * 512, 512)
                        for s in range(4):
                            ft = fb * 4 + s
                            psg, kpg = pG.get()
                            for kc in range(KC):
                                self.mm(psg[:, 0:n], wg[:, kc, s * 128:(s + 1) * 128], hT[:, kc, 0:n], kc == 0,
                                        kc == KC - 1, [kwg, khT], [kpg])
                            psb, kpb = pB.get()
                            for kc in range(4):
                                self.mm(psb[:, 0:n], wb[:, kc, s * 128:(s + 1) * 128], br[:, j, kc, 0:n], kc == 0, kc == 3,
                                        [kwb, kbr], [kpb])
                            sgt, ksg = sgp.get()
                            self.act(sgt[:, 0:n], psg[:, 0:n], AF.Sigmoid, [kpg, kmgb], [ksg], bias=mgb[:, j, ft:ft + 1])
                            if j == 0:
                                self.tt("dve", yacc[:, s, 0:n], sgt[:, 0:n], psb[:, 0:n], ALU.mult, [ksg, kpb], [kya])
                            else:
                                tm, ktm = tmp.get()
                                self.tt("dve", tm[:, 0:n], sgt[:, 0:n], psb[:, 0:n], ALU.mult, [ksg, kpb], [ktm])
                                self.tt("pool", yacc[:, s, 0:n], yacc[:, s, 0:n], tm[:, 0:n], ALU.add, [kya, ktm], [kya])
                    self.cp("act", yT[:, fb * 4:(fb + 1) * 4, 0:n], yacc[:, :, 0:n], [kya], [kyT])
                for cb in range(4):
                    wo, kwo = self.wtile(wgp, X["Wo"][l], 0, KC, cb * 512, 512)
                    for ti in range(n // 128):
                        r0 = t0 + ti * 128
                        ps, kp = pO.get()
                        for kc in range(KC):
                            self.mm(ps[:], yT[:, kc, ti * 128:(ti + 1) * 128], wo[:, kc, :], kc == 0, kc == KC - 1,
                                    [kwo, kyT], [kp])
                        xt, kx = xp.get()
                        self.ld(xt[:], self.xsrc(l, r0, 128)[:, cb * 512:(cb + 1) * 512], kx,
                                reads=["xcur"])
                        ot, ko = op_.get()
                        self.tt("dve", ot[:], ps[:], g1[:, cb * 512:(cb + 1) * 512], ALU.mult, [kp, kg1], [ko])
                        self.tt("pool", ot[:], ot[:], xt[:], ALU.add, [ko, kx], [ko])
                        self.st(X["xnext"][r0:r0 + 128, cb * 512:(cb + 1) * 512], ot[:], ko, writes=["xnext"])
            self.P.barrier()
        X["xcur"] = X["xnext"]
        self.xl = -1

    def phase_p3b(self, l):
        I, X = self.I, self.X
        L, NT = self.L, self.NT
        moe = (l % 2 == 1)
        import os
        last = (l == self.depth - 1) and not os.environ.get("NOLAST")
        xin = X["xcur"]
        xout = X["xB"] if xin is X["xA"] else X["xA"]
        nff = DFE if moe else DFF
        NFT = nff // 128
        GS = 512
        with ExitStack() as ph:
            G, kG = self.sb(ph, "G2", [128, D], F32)
            SH, kSH = self.sb(ph, "SH2", [128, D], F32)
            g2, kg2 = self.sb(ph, "g2", [128, D], F32)
            xp = self.pool(ph, "x", [128, D], F32, 1 if moe else 2)
            hp = self.pool(ph, "h", [128, D], F32, 1)
            sp_ = self.pool(ph, "ss", [128, 1], F32, 4)
            tp = self.pool(ph, "tp", [128, 512], F32, 2, psum=True)
            p1 = self.pool(ph, "p1", [128, 512], F32, 2, psum=True)
            p3 = self.pool(ph, "p3", [128, 512], F32, 2, psum=True)
            p2 = self.pool(ph, "p2", [128, 512], F32, 2, psum=True)
            hTp = self.pool(ph, "hT", [128, KC, GS], BF16, 1)
            wp = self.pool(ph, "w", [128, KC, 256], BF16, 3 if moe else 4)
            w2p = self.pool(ph, "w2", [128, 8, 512], BF16, 2 if moe else 3)
            uT, kuT = self.sb(ph, "uT", [128, NFT, GS], BF16)
            s1p = self.pool(ph, "s1", [128, GS], F32, 2)
            xqp = self.pool(ph, "xq", [128, 512], F32, 3)
            op_ = self.pool(ph, "oo", [128, 512], F32, 3)
            if moe:
                hTf = uT[:].rearrange("p a b -> p (a b)")[:, 0:2 * KC * GS].bitcast(F32).rearrange(
                    "p (k t) -> p k t", k=KC)
                khTf = kuT
                rt, krt = self.sb(ph, "rt", [128, KC, NE], F32)
                self.ld(rt[:], I["router"][:, :, :], krt)
                NTL = GS // 128
                lg, klg = self.sb(ph, "lg", [128, NTL, NE], F32)
                gw, kgw = self.sb(ph, "gw", [128, NTL, NE], F32)
                e1, ke1 = self.sb(ph, "e1", [128, NTL, NE], F32)
                e2, ke2 = self.sb(ph, "e2", [128, NTL, NE], F32)
                l2, kl2 = self.sb(ph, "l2", [128, NTL, NE], F32)
                mx, kmx = self.sb(ph, "mx", [128, NTL, 4], F32)
                acc, kacc = self.sb(ph, "acc", [128, NTL, D], F32)

            def ffn_up(W1, W3, hT, khT, n):
                for fb in range(0, nff, 256):
                    w1, kw1 = self.wtile(wp, W1, 0, KC, fb, 256)
                    w3, kw3 = self.wtile(wp, W3, 0, KC, fb, 256)
                    for s_ in range(2):
                        ft = fb // 128 + s_
                        ps1, k1 = p1.get()
                        for kc in range(KC):
                            self.mm(ps1[:, 0:n], w1[:, kc, s_ * 128:(s_ + 1) * 128], hT[:, kc, 0:n], kc == 0,
                                    kc == KC - 1, [kw1, khT], [k1])
                        ps3, k3 = p3.get()
                        for kc in range(KC):
                            self.mm(ps3[:, 0:n], w3[:, kc, s_ * 128:(s_ + 1) * 128], hT[:, kc, 0:n], kc == 0,
                                    kc == KC - 1, [kw3, khT], [k3])
                        s1, ks1 = s1p.get()
                        self.act(s1[:, 0:n], ps1[:, 0:n], AF.Silu, [k1], [ks1])
                        self.tt("dve", uT[:, ft, 0:n], s1[:, 0:n], ps3[:, 0:n], ALU.mult, [ks1, k3], [kuT])

            def ffn_down(W2, n, cb, emit):
                nt = n // 128
                for half in range(0, nt, 2):
                    tis = list(range(half, min(nt, half + 2)))
                    pss = [p2.get() for _ in tis]
                    for k0 in range(0, NFT, 8):
                        nk = min(8, NFT - k0)
                        w2, kw2 = self.wtile(w2p, W2, k0, nk, cb * 512, 512)
                        for ii, ti in enumerate(tis):
                            ps, kp = pss[ii]
                            for kc in range(nk):
                                self.mm(ps[:], uT[:, k0 + kc, ti * 128:(ti + 1) * 128], w2[:, kc, :], k0 + kc == 0,
                                        k0 + kc == NFT - 1, [kuT, kw2], [kp], inc=(kc == nk - 1))
                    for ii, ti in enumerate(tis):
                        emit(ti, pss[ii][0], pss[ii][1])

            cur_which = None
            for (t0, n, isctx) in self.groups(self.need_ctx, GS):
                which = 1 if isctx else 0
                if which != cur_which:
                    cur_which = which
                    ngt, kng = xp.get()
                    self.ld(ngt[:], I["n2g"][l, :].partition_broadcast(128), kng)
                    self.load_vec(G, kG, l, which, 4, (ngt, kng))
                    self.load_vec(SH, kSH, l, which, 3)
                    self.load_vec(g2, kg2, l, which, 5)
                hT, khT = hTp.get()
                self.norm_group(lambda a, b: xin[a:a + b, :], t0, n, G, kG, SH, kSH, xp, hp, sp_, tp, hT, khT,
                                hTf=(hTf, khTf) if (moe and not os.environ.get("NOHTF")) else None)
                nt = n // 128

                def final(ti, cb, src, ksrc):
                    r0 = t0 + ti * 128
                    xq, kxq = xqp.get()
                    self.ld(xq[:], xin[r0:r0 + 128, cb * 512:(cb + 1) * 512], kxq)
                    ot, ko = op_.get()
                    self.tt("dve", ot[:], src, g2[:, cb * 512:(cb + 1) * 512], ALU.mult, [ksrc, kg2], [ko])
                    self.tt("pool", ot[:], ot[:], xq[:], ALU.add, [ko, kxq], [ko])
                    if last:
                        self.st(self.out[r0 - L:r0 - L + 128, cb * 512:(cb + 1) * 512], ot[:], ko, q="sp")
                    else:
                        self.st(xout[r0:r0 + 128, cb * 512:(cb + 1) * 512], ot[:], ko, writes=["xcur"])

                if not moe:
                    ffn_up(X["F1"], X["F3"], hT, khT, n)
                    for cb in range(4):
                        ffn_down(X["F2"], n, cb, lambda ti, ps, kp, cb=cb: final(ti, cb, ps[:], kp))
                else:
                    import os
                    LV = int(os.environ.get("MOE_LEVEL", "3"))
                    if LV < 3 and not os.environ.get("NOMEMSET"):
                        self.memset("dve", acc[:], 0.0, [kacc])
                    for ti in (range(nt) if not os.environ.get("NOROUTER") else []):
                        ps, kp = p2.get()
                        for kc in range(KC):
                            self.mm(ps[:, 0:NE], hTf[:, kc, ti * 128:(ti + 1) * 128], rt[:, kc, :], kc == 0, kc == KC - 1,
                                    [khTf, krt], [kp])
                        self.cp("dve", lg[:, ti, :], ps[:, 0:NE], [kp], [klg])
                    if LV >= 2:
                      self.red(mx[:, 0:nt, 0:1].rearrange("p t o -> p (t o)"), lg[:, 0:nt, :], ALU.max, [klg], [kmx])
                      self.tt("dve", e1[:, 0:nt, :], lg[:, 0:nt, :], mx[:, 0:nt, 0:1].broadcast_to([128, nt, NE]),
                            ALU.is_equal, [klg, kmx], [ke1])
                    for _lv in ([1] if LV >= 2 else []):
                     self.stt("dve", l2[:, 0:nt, :], e1[:, 0:nt, :], -1e30, lg[:, 0:nt, :], ALU.mult, ALU.add, [ke1, klg],
                             [kl2])
                     self.red(mx[:, 0:nt, 1:2].rearrange("p t o -> p (t o)"), l2[:, 0:nt, :], ALU.max, [kl2], [kmx])
                     self.tt("dve", e2[:, 0:nt, :], l2[:, 0:nt, :], mx[:, 0:nt, 1:2].broadcast_to([128, nt, NE]),
                             ALU.is_equal, [kl2, kmx], [ke2])
                     self.tt("dve", mx[:, 0:nt, 2:3], mx[:, 0:nt, 1:2], mx[:, 0:nt, 0:1], ALU.subtract, [kmx], [kmx])
                     self.act(mx[:, 0:nt, 2:3], mx[:, 0:nt, 2:3], AF.Exp, [kmx], [kmx])
                     self.ts("dve", mx[:, 0:nt, 3:4], mx[:, 0:nt, 2:3], 1.0, None, ALU.add, None, [kmx], [kmx])
                     self.recip(mx[:, 0:nt, 3:4], mx[:, 0:nt, 3:4], [kmx], [kmx])
                     self.tt("dve", mx[:, 0:nt, 2:3], mx[:, 0:nt, 2:3], mx[:, 0:nt, 3:4], ALU.mult, [kmx], [kmx])
                     self.tt("dve", gw[:, 0:nt, :], e1[:, 0:nt, :], mx[:, 0:nt, 3:4].broadcast_to([128, nt, NE]), ALU.mult,
                             [ke1, kmx], [kgw])
                     self.tt("dve", e2[:, 0:nt, :], e2[:, 0:nt, :], mx[:, 0:nt, 2:3].broadcast_to([128, nt, NE]), ALU.mult,
                             [ke2, kmx], [ke2])
                     self.tt("dve", gw[:, 0:nt, :], gw[:, 0:nt, :], e2[:, 0:nt, :], ALU.add, [kgw, ke2], [kgw])
                    if LV >= 2:
                        self.st(X["gws"][t0:t0 + n, :].rearrange("(t p) e -> p t e", p=128), gw[:, 0:nt, :], kgw,
                                writes=["gws"])
                    for e in (range(NE) if LV >= 3 else []):
                        ffn_up(X["M1"][e], X["M3"][e], hT, khT, n)
                        for cb in range(4):
                            def emit(ti, ps, kp, e=e, cb=cb):
                                dst = acc[:, ti, cb * 512:(cb + 1) * 512]
                                if e == 0:
                                    self.ts("dve", dst, ps[:], gw[:, ti, e:e + 1], None, ALU.mult, None, [kp, kgw], [kacc])
                                else:
                                    self.stt("dve", dst, ps[:], gw[:, ti, e:e + 1], dst, ALU.mult, ALU.add,
                                             [kp, kgw, kacc], [kacc])
                            ffn_down(X["M2"][e], n, cb, emit)
                    for ti in range(nt):
                        for cb in range(4):
                            final(ti, cb, acc[:, ti, cb * 512:(cb + 1) * 512], kacc)
            self.P.barrier()
        X["xcur"] = xout


def rope_tables(S, hd):
    r = hd // 4
    t = np.arange(S)
    rows = (t // 64).astype(np.float32)
    cols = (t % 64).astype(np.float32)
    freqs = (10000.0 ** (-np.arange(r, dtype=np.float32) / r)).astype(np.float32)
    ang = np.concatenate([rows[:, None] * freqs, cols[:, None] * freqs], -1).astype(np.float32)
    ang = ang.reshape(S, 2, r)
    cos = np.cos(ang).astype(np.float32)
    sin = np.sin(ang).astype(np.float32)
    C = np.zeros((S, 2, 2, r), np.float32)
    Sg = np.zeros((S, 2, 2, r), np.float32)
    C[:, :, 0, :] = cos
    C[:, :, 1, :] = cos
    Sg[:, :, 0, :] = -sin
    Sg[:, :, 1, :] = sin
    idx = np.arange(hd).reshape(2, 2, r)
    partner = np.stack([idx[:, 1, :], idx[:, 0, :]], 1).reshape(hd)
    Pm = np.zeros((hd, hd), np.float32)
    Pm[partner, np.arange(hd)] = 1.0
    return (np.ascontiguousarray(C.reshape(S, hd).T), np.ascontiguousarray(Sg.reshape(S, hd).T), Pm)


def make_inputs(b, S, L, inp):
    f = lambda a: np.ascontiguousarray(a, dtype=np.float32)
    m = {}
    m["x"] = f(inp["x"][b])
    m["ctx"] = f(inp["ctx"][b])
    cc = np.stack([inp["c"][b], inp["c_ctx"]], 0)
    m["ccT"] = f(cc.T.reshape(KC, 128, 2).transpose(1, 0, 2))
    m["ada_w"] = f(inp["ada_w"])
    m["ada_b"] = f(inp["ada_b"])
    m["w_in"] = f(inp["w_in"])
    m["mg_w"] = f(inp["merge_gate_w"])
    m["br_w"] = f(inp["branch_w"])
    m["out_w"] = f(inp["out_w"])
    m["ffn_w1"] = f(inp["ffn_w1"])
    m["ffn_w3"] = f(inp["ffn_w3"])
    m["ffn_w2"] = f(inp["ffn_w2"])
    m["router"] = f(inp["moe_router"][0].reshape(KC, 128, NE).transpose(1, 0, 2))
    m["moe_w1"] = f(inp["moe_w1"])
    m["moe_w3"] = f(inp["moe_w3"])
    m["moe_w2"] = f(inp["moe_w2"])
    m["n1g"] = f(inp["norm1_g"])
    m["n2g"] = f(inp["norm2_g"])
    cw = inp["mlstm_conv_w"]
    cb = inp["mlstm_conv_b"]
    mcv = np.concatenate([cw.transpose(0, 2, 1), cb[:, :, None]], -1)
    m["mcv"] = f(mcv.reshape(2, 8, 128, 4))
    m["mgb"] = f(np.broadcast_to(inp["mlstm_gate_b"][:, None, :], (2, 64, 16)))
    m["mng"] = f(inp["mlstm_norm_g"])
    dw = inp["conv_dw_w"]
    cd = np.concatenate([dw.transpose(0, 2, 1), inp["conv_dw_b"][:, :, None], inp["conv_ln_g"][:, :, None],
                         inp["conv_ln_b"][:, :, None]], -1)
    m["cdw"] = f(cd.reshape(2, 4, 128, 34))
    m["gqg"] = f(np.stack([inp["gqa_q_norm_g"], inp["gqa_k_norm_g"]], -1))
    m["dqg"] = f(np.stack([inp["diff_q_norm_g"], inp["diff_k_norm_g"]], -1))
    m["dlam"] = f(inp["diff_lambda"].reshape(2, 256))
    m["dsg"] = f(inp["diff_subln_g"])
    mb = inp["merge_gate_b"]
    m["mgbias"] = f(mb.reshape(2, 4, 16, 128).transpose(0, 3, 1, 2).reshape(2, 128, 64))
    m["ident"] = np.eye(128, dtype=np.float32)
    tri = np.triu(np.ones((64, 64), np.float32))
    m["triF"] = tri
    m["triB"] = np.ascontiguousarray(tri.T)
    m["eye64"] = np.eye(64, dtype=np.float32)
    cG, sG, pG = rope_tables(S, 128)
    cD, sD, pD = rope_tables(S, 64)
    m["cosG"], m["sinG"], m["swapG"] = cG, sG, pG
    m["cosD"], m["sinD"], m["swapD"] = cD, sD, pD
    return m


_CACHE = {}


def kernel(**inputs):
    inp = {k: np.asarray(v) for k, v in inputs.items()}
    B, S, _ = inp["x"].shape
    L = inp["ctx"].shape[1]
    key = (S, L)
    if key not in _CACHE:
        _CACHE[key] = Builder(S, L, 2).build()
    nc = _CACHE[key]
    in_maps = [make_inputs(b, S, L, inp) for b in range(B)]
    res = run_bass_kernel_spmd(nc, in_maps, core_ids=list(range(B)))
    return np.stack([np.asarray(r["out"]) for r in res.results], 0).astype(np.float32)
```
